# Optimizing a Trainium2 kernel written in Bass

```python
import jax, jax.numpy as jnp
from jax import lax
import numpy as np

D_MODEL = 1024
BATCH = 4
SEQ = 4096
DEPTH = 1

HEAD_DIM = 64
MIX_WIDTH = D_MODEL
A_WIDTH = MIX_WIDTH // 2
B_WIDTH = MIX_WIDTH - A_WIDTH
A_HEADS = A_WIDTH // HEAD_DIM
B_Q_HEADS = B_WIDTH // HEAD_DIM
B_KV_HEADS = 2
B_KV_WIDTH = B_KV_HEADS * HEAD_DIM
IN_WIDTH = 3 * A_WIDTH + B_WIDTH + 2 * B_KV_WIDTH
A_CONFIGS = ((128, 1), (512, 4), (2048, 16))
A_BLOCK = 64
B_HALF_WINDOW = 128
B_BLOCK = 128
N_GROUPS = 4
EXPERTS_PER_GROUP = 8
N_EXPERTS = N_GROUPS * EXPERTS_PER_GROUP
TOP_K = 2
D_EXPERT = D_MODEL // 2
PLE_DIM = 256
EPS = 1e-6
NEG = -1e30

kernel_name = "hybrid_dilated_swa_hiermoe_encoder"


def rmsnorm(x, g):
    xf = x.astype(jnp.float32)
    r = lax.rsqrt(jnp.mean(xf * xf, axis=-1, keepdims=True) + EPS)
    return (xf * r).astype(x.dtype) * g


def alibi_slopes(n):
    return jnp.exp2(-8.0 * jnp.arange(1, n + 1, dtype=jnp.float32) / n)


def banded_attention(q, k, v, half, blk, slopes, dist_scale, sink=None):
    n, l, hk, g, dh = q.shape
    nb = -(-l // blk)
    lp = nb * blk
    q = jnp.pad(q, ((0, 0), (0, lp - l), (0, 0), (0, 0), (0, 0)))
    pad_kv = ((0, 0), (blk, lp - l + blk), (0, 0), (0, 0))
    kb = jnp.pad(k, pad_kv).reshape(n, nb + 2, blk, hk, dh)
    vb = jnp.pad(v, pad_kv).reshape(n, nb + 2, blk, hk, dh)
    kw = jnp.concatenate([kb[:, :-2], kb[:, 1:-1], kb[:, 2:]], axis=2)
    vw = jnp.concatenate([vb[:, :-2], vb[:, 1:-1], vb[:, 2:]], axis=2)
    qb = q.reshape(n, nb, blk, hk, g, dh)
    s = jnp.einsum('nbqhgd,nbkhd->nbhgqk', qb, kw,
                   preferred_element_type=jnp.float32) * (dh ** -0.5)
    qpos = jnp.arange(lp).reshape(nb, blk)
    kpos = (jnp.arange(nb)[:, None] - 1) * blk + jnp.arange(3 * blk)[None, :]
    rel = kpos[:, None, :] - qpos[:, :, None]
    valid = (jnp.abs(rel) <= half) & (kpos[:, None, :] >= 0) & (kpos[:, None, :] < l)
    dist = jnp.abs(rel).astype(jnp.float32) * dist_scale
    s = s - slopes.astype(jnp.float32)[:, :, None, None] * dist[None, :, None, None]
    s = jnp.where(valid[None, :, None, None], s, NEG)
    m = jnp.max(s, axis=-1)
    if sink is not None:
        sk = sink.astype(jnp.float32)[:, :, None]
        m = jnp.maximum(m, sk)
    pexp = jnp.exp(s - m[..., None])
    den = jnp.sum(pexp, axis=-1)
    if sink is not None:
        den = den + jnp.exp(sk - m)
    o = jnp.einsum('nbhgqk,nbkhd->nbqhgd', pexp.astype(v.dtype), vw,
                   preferred_element_type=jnp.float32)
    o = o / den.transpose(0, 1, 4, 2, 3)[..., None]
    o = o.reshape(n, lp, hk, g, dh)[:, :l]
    lse = (m + jnp.log(den)).transpose(0, 1, 4, 2, 3).reshape(n, lp, hk, g)[:, :l]
    return o.astype(q.dtype), lse


def dilated_attention(q, k, v, slopes):
    b, s, h, dh = q.shape
    outs, lses = [], []
    for window, dil in A_CONFIGS:
        l = s // dil

        def to_sub(t):
            return t.reshape(b, l, dil, h, dh).transpose(0, 2, 1, 3, 4).reshape(b * dil, l, h, dh)

        o, lse = banded_attention(to_sub(q)[:, :, :, None], to_sub(k), to_sub(v),
                                  window // (2 * dil), A_BLOCK, slopes[:, None], float(dil))
        o = o[:, :, :, 0].reshape(b, dil, l, h, dh).transpose(0, 2, 1, 3, 4).reshape(b, s, h, dh)
        lse = lse[..., 0].reshape(b, dil, l, h).transpose(0, 2, 1, 3).reshape(b, s, h)
        outs.append(o)
        lses.append(lse)
    w = jax.nn.softmax(jnp.stack(lses, axis=0), axis=0)
    o = jnp.einsum('cbsh,cbshd->bshd', w, jnp.stack(outs, axis=0).astype(jnp.float32))
    return o.astype(q.dtype)


def windowed_gqa_sink(q, k, v, sink):
    b, s, hq, dh = q.shape
    g = hq // B_KV_HEADS
    qg = q.reshape(b, s, B_KV_HEADS, g, dh)
    slopes = alibi_slopes(hq).reshape(B_KV_HEADS, g)
    o, _ = banded_attention(qg, k, v, B_HALF_WINDOW, B_BLOCK, slopes, 1.0,
                            sink.reshape(B_KV_HEADS, g))
    return o.reshape(b, s, hq * dh)


def hier_moe(h, w_rg, b_rg, w_re, b_re, w_gate, w_up, w_down):
    b, s, d = h.shape
    t = h.reshape(b * s, d)
    n_tok = t.shape[0]
    glog = (t @ w_rg).astype(jnp.float32) + b_rg.astype(jnp.float32)
    gsel = jnp.argmax(glog, axis=-1)
    gw = jnp.max(jax.nn.softmax(glog, axis=-1), axis=-1)
    elog = ((t @ w_re).astype(jnp.float32) + b_re.astype(jnp.float32)).reshape(
        n_tok, N_GROUPS, EXPERTS_PER_GROUP)
    elog_sel = elog[jnp.arange(n_tok), gsel]
    top_v, top_i = lax.top_k(elog_sel, TOP_K)
    top_w = jax.nn.softmax(top_v, axis=-1) * gw[:, None]
    eid = gsel[:, None] * EXPERTS_PER_GROUP + top_i
    dense_w = jnp.sum(jax.nn.one_hot(eid, N_EXPERTS, dtype=jnp.float32) * top_w[..., None], axis=1)
    out = jnp.zeros((n_tok, d), jnp.float32)
    for grp in range(N_GROUPS):
        sl = slice(grp * EXPERTS_PER_GROUP, (grp + 1) * EXPERTS_PER_GROUP)
        a = jnp.einsum('td,edf->tef', t, w_gate[sl])
        u = jnp.einsum('td,edf->tef', t, w_up[sl])
        hid = jax.nn.silu(a) * u * dense_w[:, sl, None].astype(t.dtype)
        out = out + jnp.einsum('tef,efd->td', hid, w_down[sl], preferred_element_type=jnp.float32)
    return out.astype(h.dtype).reshape(b, s, d)


def setup_inputs(seed: int = 0) -> dict:
    key = jax.random.key(seed)
    ks = jax.random.split(key, 20)
    f32 = jnp.float32

    def nrm(k, shape, scale):
        return jax.random.normal(k, shape, f32) * scale

    def gain(k, shape):
        return 1.0 + 0.05 * jax.random.normal(k, shape, f32)

    return {
        "x": nrm(ks[0], (BATCH, SEQ, D_MODEL), 1.0),
        "p": nrm(ks[1], (DEPTH, BATCH, SEQ, PLE_DIM), 1.0),
        "g_mix": gain(ks[2], (DEPTH, D_MODEL)),
        "w_in": nrm(ks[3], (DEPTH, D_MODEL, IN_WIDTH), D_MODEL ** -0.5),
        "sink": nrm(ks[4], (DEPTH, B_Q_HEADS), 0.5),
        "g_grp_a": gain(ks[5], (DEPTH, A_WIDTH)),
        "g_grp_b": gain(ks[6], (DEPTH, B_WIDTH)),
        "w_out": nrm(ks[7], (DEPTH, MIX_WIDTH, D_MODEL), MIX_WIDTH ** -0.5),
        "g_ffn": gain(ks[8], (DEPTH, D_MODEL)),
        "w_router_group": nrm(ks[9], (DEPTH, D_MODEL, N_GROUPS), D_MODEL ** -0.5),
        "b_router_group": nrm(ks[10], (DEPTH, N_GROUPS), 0.01),
        "w_router_expert": nrm(ks[11], (DEPTH, D_MODEL, N_EXPERTS), D_MODEL ** -0.5),
        "b_router_expert": nrm(ks[12], (DEPTH, N_EXPERTS), 0.01),
        "w_expert_gate": nrm(ks[13], (DEPTH, N_EXPERTS, D_MODEL, D_EXPERT), D_MODEL ** -0.5),
        "w_expert_up": nrm(ks[14], (DEPTH, N_EXPERTS, D_MODEL, D_EXPERT), D_MODEL ** -0.5),
        "w_expert_down": nrm(ks[15], (DEPTH, N_EXPERTS, D_EXPERT, D_MODEL), D_EXPERT ** -0.5),
        "g_ple": gain(ks[16], (DEPTH, D_MODEL)),
        "w_ple_gate": nrm(ks[17], (DEPTH, D_MODEL, D_MODEL), D_MODEL ** -0.5),
        "w_ple_proj": nrm(ks[18], (DEPTH, PLE_DIM, D_MODEL), PLE_DIM ** -0.5),
        "g_final": gain(ks[19], (D_MODEL,)),
    }


def reference(x, p, g_mix, w_in, sink, g_grp_a, g_grp_b, w_out, g_ffn,
              w_router_group, b_router_group, w_router_expert, b_router_expert,
              w_expert_gate, w_expert_up, w_expert_down, g_ple, w_ple_gate, w_ple_proj,
              g_final):
    b, s, _ = x.shape
    split_at = [A_WIDTH, 2 * A_WIDTH, 3 * A_WIDTH, 3 * A_WIDTH + B_WIDTH,
                3 * A_WIDTH + B_WIDTH + B_KV_WIDTH]
    slopes_a = alibi_slopes(A_HEADS)
    for i in range(DEPTH):
        h = rmsnorm(x, g_mix[i])
        proj = h @ w_in[i]
        qa, ka, va, qb, kb, vb = jnp.split(proj, split_at, axis=-1)
        oa = dilated_attention(qa.reshape(b, s, A_HEADS, HEAD_DIM),
                               ka.reshape(b, s, A_HEADS, HEAD_DIM),
                               va.reshape(b, s, A_HEADS, HEAD_DIM), slopes_a).reshape(b, s, A_WIDTH)
        ob = windowed_gqa_sink(qb.reshape(b, s, B_Q_HEADS, HEAD_DIM),
                               kb.reshape(b, s, B_KV_HEADS, HEAD_DIM),
                               vb.reshape(b, s, B_KV_HEADS, HEAD_DIM), sink[i])
        mix = jnp.concatenate([rmsnorm(oa, g_grp_a[i]), rmsnorm(ob, g_grp_b[i])], axis=-1)
        x = x + mix @ w_out[i]
        h = rmsnorm(x, g_ffn[i])
        x = x + hier_moe(h, w_router_group[i], b_router_group[i], w_router_expert[i],
                         b_router_expert[i], w_expert_gate[i], w_expert_up[i], w_expert_down[i])
        gate = jax.nn.sigmoid(rmsnorm(x, g_ple[i]) @ w_ple_gate[i])
        x = x + (p[i] @ w_ple_proj[i]) * gate
    return rmsnorm(x, g_final)
```

```python
import os
from contextlib import ExitStack
import numpy as np
import concourse.bass as bass
import concourse.mybir as mybir
from concourse.bass_utils import run_bass_kernel_spmd

F32 = mybir.dt.float32
BF16 = mybir.dt.bfloat16
I32 = mybir.dt.int32
U8 = mybir.dt.uint8
AF = mybir.ActivationFunctionType
ALU = mybir.AluOpType
AX = mybir.AxisListType

NTOK = 2048
NLOC = 3072
EPS = 1e-6
ARENA = 209920


class Buf:
    __slots__ = ("name", "w", "r")

    def __init__(self, name):
        self.name = name
        self.w = None
        self.r = {}


class Sched:
    ENG = ("pe", "act", "dve", "pool", "sp")

    def __init__(self, nc, stack):
        self.nc = nc
        self.stack = stack
        self.sems = {}
        self.cnt = {}
        self.seen = {e: {} for e in self.ENG}
        self.prog = {e: [] for e in self.ENG}
        for e in ("pe", "act", "dve", "pool"):
            self.sems[e] = stack.enter_context(nc.semaphore("s_" + e))
            self.cnt[e] = 0

    def _need(self, eng, toks):
        need = {}
        for t in toks:
            if t is None:
                continue
            k, v = t
            if k == eng and eng == "pe":
                continue
            if self.seen[eng].get(k, 0) >= v:
                continue
            if need.get(k, 0) < v:
                need[k] = v
        for k, v in need.items():
            self.seen[eng][k] = v
        return list(need.items())

    @staticmethod
    def _deps(reads, writes):
        toks = []
        for b in reads:
            toks.append(b.w)
        for b in writes:
            toks.append(b.w)
            for k, v in b.r.items():
                toks.append((k, v))
        return toks

    def op(self, eng, fn, reads=(), writes=()):
        waits = self._need(eng, self._deps(reads, writes))
        self.cnt[eng] += 1
        tok = (eng, self.cnt[eng])
        self._emit_now(eng, waits, fn, (eng, 1))
        for b in reads:
            if b.r.get(eng, 0) < tok[1]:
                b.r[eng] = tok[1]
        for b in writes:
            b.w = tok
            b.r = {}
        return tok

    def dma(self, q, fn, reads=(), writes=(), sem=None):
        if sem is None:
            sem = "d_" + (writes[0].name if writes else reads[0].name)
        if sem not in self.sems:
            self.sems[sem] = self.stack.enter_context(self.nc.semaphore(sem))
            self.cnt[sem] = 0
        waits = self._need(q, self._deps(reads, writes))
        self.cnt[sem] += 16
        tok = (sem, self.cnt[sem])
        self._emit_now(q, waits, fn, (sem, 16))
        for b in reads:
            if b.r.get(sem, 0) < tok[1]:
                b.r[sem] = tok[1]
        for b in writes:
            b.w = tok
            b.r = {}
        return tok

    def _emit_now(self, eng, waits, fn, inc):
        nc = self.nc
        eo = {"pe": nc.tensor, "act": nc.scalar, "dve": nc.vector, "pool": nc.gpsimd, "sp": nc.sync}[eng]
        for k, v in waits:
            eo.wait_ge(self.sems[k], v)
        if fn is not None:
            fn(eo).then_inc(self.sems[inc[0]], inc[1])

    def barrier(self):
        toks = [(k, v) for k, v in self.cnt.items() if v > 0]
        for e in self.ENG:
            waits = self._need(e, toks)
            if waits:
                self._emit_now(e, waits, None, None)

    def emit(self):
        return


class Arena:
    def __init__(self, nc, nbytes):
        self.t = nc.alloc_sbuf_tensor("arena", [128, nbytes], U8)
        self.n = nbytes
        self.top = 0

    def alloc(self, free, dtype):
        es = 4 if dtype in (F32, I32) else 2
        n = es
        for s in free:
            n *= s
        off = (self.top + 63) // 64 * 64
        assert off + n <= self.n, ("arena overflow", off + n, self.n)
        self.top = off + n
        v = self.t[:, off:off + n].bitcast(dtype)
        if len(free) == 2:
            v = v.rearrange("p (a b) -> p a b", a=free[0])
        elif len(free) == 3:
            v = v.rearrange("p (a b c) -> p a b c", a=free[0], b=free[1])
        return v


def build_nc(stage=99, dbg=False):
    nc = bass.Bass("TRN2", target_bir_lowering=False)

    def din(name, shape):
        return nc.dram_tensor(name, list(shape), F32, kind="ExternalInput").ap()

    xl = din("xl", [NLOC, 1024])
    pl = din("pl", [NTOK, 256])
    g_mix = din("g_mix", [1024])
    w_in = din("w_in", [1024, 2304])
    sink = din("sink", [8])
    g_grp_a = din("g_grp_a", [512])
    g_grp_b = din("g_grp_b", [512])
    w_out = din("w_out", [1024, 1024])
    g_ffn = din("g_ffn", [1024])
    w_rg = din("w_rg", [1024, 4])
    b_rg = din("b_rg", [4])
    w_re = din("w_re", [1024, 32])
    b_re = din("b_re", [32])
    if stage >= 3:
        w_eg = din("w_eg", [32, 1024, 512])
        w_eu = din("w_eu", [32, 1024, 512])
        w_ed = din("w_ed", [32, 512, 1024])
    g_ple = din("g_ple", [1024])
    w_pg = din("w_pg", [1024, 1024])
    w_pp = din("w_pp", [256, 1024])
    g_final = din("g_final", [1024])
    y = nc.dram_tensor("y", [NTOK, 1024], F32, kind="ExternalOutput").ap()
    dbg_o = None
    if dbg:
        dbg_o = nc.dram_tensor("dbg", [NTOK, 1024], F32, kind="ExternalOutput").ap()

    with ExitStack() as stack:
        S = Sched(nc, stack)
        A = Arena(nc, ARENA)
        ps = nc.alloc_psum_tensor("ps", [128, 8, 512], F32)
        b_ps = [Buf("ps%d" % i) for i in range(8)]

        def psb(i):
            return ps[:, i, :]

        def psb16(i):
            return ps[:, i, :].bitcast(BF16)

        ident_f = A.alloc((128,), F32)
        ident_b = A.alloc((128,), BF16)
        gall = A.alloc((128,), F32)
        gT = A.alloc((32,), F32)
        wr = A.alloc((8, 36), F32)
        rbias = A.alloc((36,), F32)
        esink = A.alloc((8,), F32)
        junk4 = A.alloc((1024,), BF16)
        st4 = A.alloc((6, 16), F32)
        stm = A.alloc((3, 16), F32)
        b_junk4, b_st4, b_stm = Buf("junk4"), Buf("st4"), Buf("stm")
        junk3 = A.alloc((512,), BF16)
        st3 = A.alloc((3, 32), F32)
        b_junk3, b_st3 = Buf("junk3"), Buf("st3")
        dw = A.alloc((16, 32), F32)
        b_ident, b_gall, b_gT, b_wr, b_rbias, b_esink, b_gfin = [Buf(n) for n in
            ("ident", "gall", "gT", "wr", "rbias", "esink", "gfin")]
        b_dw = [Buf("dw%d" % t) for t in range(16)]

        S.op("pool", lambda e: e.memset(ident_f, 0.0), writes=[b_ident])
        S.op("pool", lambda e: e.affine_select(out=ident_f, in_=ident_f, pattern=[[-1, 128]],
                                               compare_op=ALU.not_equal, fill=1.0, base=0, channel_multiplier=1),
             reads=[b_ident], writes=[b_ident])
        S.op("dve", lambda e: e.tensor_copy(out=ident_b, in_=ident_f), reads=[b_ident], writes=[b_ident])
        S.op("pool", lambda e: e.memset(gall, 0.0), writes=[b_gall])
        for i, (g, nk) in enumerate(((g_mix, 8), (g_ffn, 8), (g_ple, 8))):
            S.dma("sp", lambda e, g=g, i=i: e.dma_start(out=gall[8 * i:8 * i + 8, :],
                                                        in_=g.rearrange("(k p) -> k p", p=128)),
                  writes=[b_gall])
        S.dma("sp", lambda e: e.dma_start(out=gall[24:28, :], in_=g_grp_a.rearrange("(k p) -> k p", p=128)),
              writes=[b_gall])
        S.dma("sp", lambda e: e.dma_start(out=gall[28:32, :], in_=g_grp_b.rearrange("(k p) -> k p", p=128)),
              writes=[b_gall])
        S.dma("sp", lambda e: e.dma_start(out=wr[:, :, 0:4], in_=w_rg.rearrange("(k p) g -> p k g", p=128)),
              writes=[b_wr])
        S.dma("sp", lambda e: e.dma_start(out=wr[:, :, 4:36], in_=w_re.rearrange("(k p) g -> p k g", p=128)),
              writes=[b_wr])
        S.dma("sp", lambda e: e.dma_start(out=rbias[:, 0:4], in_=b_rg.partition_broadcast(128)),
              writes=[b_rbias])
        S.dma("sp", lambda e: e.dma_start(out=rbias[:, 4:36], in_=b_re.partition_broadcast(128)),
              writes=[b_rbias])
        S.dma("sp", lambda e: e.dma_start(out=esink, in_=sink.partition_broadcast(128)),
              writes=[b_esink])
        S.op("act", lambda e: e.activation(out=esink, in_=esink, func=AF.Exp), reads=[b_esink], writes=[b_esink])
        S.op("pe", lambda e: e.transpose(out=psb(0)[:, 0:32], in_=gall[0:32, :], identity=ident_f[0:32, 0:32]),
             reads=[b_gall, b_ident], writes=[b_ps[0]])
        S.op("dve", lambda e: e.tensor_copy(out=gT, in_=psb(0)[:, 0:32]), reads=[b_ps[0]], writes=[b_gT])

        base = A.top

        def rstd_ops(ssq, tmp, out, n, bufs):
            S.op("dve", lambda e: e.tensor_scalar(out=tmp, in0=ssq, scalar1=1.0 / n, scalar2=EPS,
                                                  op0=ALU.mult, op1=ALU.add), reads=bufs, writes=bufs)
            S.op("act", lambda e: e.activation(out=tmp, in_=tmp, func=AF.Sqrt), reads=bufs, writes=bufs)
            S.op("dve", lambda e: e.reciprocal(out=out, in_=tmp), reads=bufs, writes=bufs)

        R1 = A.alloc((65536 // 2,), BF16)
        QT_A = R1[:, 0:8192].rearrange("p (a b) -> p a b", a=4)
        KT_A = R1[:, 8192:8192 + 12288].rearrange("p (a b) -> p a b", a=4)
        VT_A = R1[:, 20480:20480 + 12288].rearrange("p (a b) -> p a b", a=4)
        x1 = R1.bitcast(F32).rearrange("p (t d) -> p t d", t=16)
        r2_off = A.top
        QT_B = A.alloc((4, 2048), BF16)
        KT_B = A.alloc((2, 2176), BF16)
        VT_B = A.alloc((2176,), BF16)
        r3_off = A.top
        b_QA, b_KA, b_VA, b_QB, b_KB, b_VB = [Buf(n) for n in ("QA", "KA", "VA", "QB", "KB", "VB")]

        hT = A.alloc((8, NLOC), BF16)
        xs = [A.alloc((1024,), F32) for _ in range(2)]
        xn = [A.alloc((1024,), BF16) for _ in range(2)]
        junk = A.alloc((1024,), BF16)
        wblk = [A.alloc((8, 768), BF16) for _ in range(2)]
        st1 = A.alloc((24, 4), F32)
        wkb = A.alloc((8, 2, 128), BF16)
        b_wkb = Buf("wkb")
        for g_ in range(2):
            for hh_ in range(2):
                S.dma("pool", lambda e, g_=g_, hh_=hh_: e.dma_start(
                    out=wkb[:, :, g_, 64 * hh_:64 * hh_ + 64],
                    in_=w_in[:, 2048 + 64 * g_:2048 + 64 * g_ + 64].rearrange("(k p) c -> p k c", p=128)),
                    writes=[b_wkb])
        b_hT = [Buf("hT%d" % i) for i in range(6)]
        b_xs = [Buf("xs%d" % i) for i in range(2)]
        b_xn = [Buf("xn%d" % i) for i in range(2)]
        b_junk = Buf("junk")
        b_wblk = [Buf("wblk%d" % i) for i in range(2)]
        b_st1 = [Buf("st1_%d" % i) for i in range(24)]

        def load_wblk(bi):
            S.dma("pool", lambda e, bi=bi: e.dma_start(
                out=wblk[bi % 2], in_=w_in[:, 768 * bi:768 * (bi + 1)].rearrange("(k p) c -> p k c", p=128)),
                writes=[b_wblk[bi % 2]])

        load_wblk(0)
        load_wblk(1)

        gmb = gT[:, 0:8].unsqueeze(2).to_broadcast([128, 8, 128])
        for t in range(24):
            s = t % 2
            S.dma("sp", lambda e, t=t, s=s: e.dma_start(out=xs[s], in_=xl[128 * t:128 * (t + 1), :]),
                  writes=[b_xs[s]])
            S.op("act", lambda e, t=t, s=s: e.activation(out=junk, in_=xs[s], func=AF.Square,
                                                         accum_out=st1[:, t, 0:1]),
                 reads=[b_xs[s]], writes=[b_junk, b_st1[t]])
            rstd_ops(st1[:, t, 0:1], st1[:, t, 1:2], st1[:, t, 2:3], 1024, [b_st1[t]])
            S.op("dve", lambda e, t=t, s=s: e.tensor_scalar(out=xn[s], in0=xs[s], scalar1=st1[:, t, 2:3],
                                                            scalar2=None, op0=ALU.mult),
                 reads=[b_xs[s], b_st1[t]], writes=[b_xn[s]])
            pb = t % 2
            for k in range(8):
                S.op("pe", lambda e, k=k, s=s, pb=pb: e.transpose(out=psb16(pb)[:, 128 * k:128 * (k + 1)],
                                                                  in_=xn[s][:, 128 * k:128 * (k + 1)],
                                                                  identity=ident_b),
                     reads=[b_xn[s], b_ident], writes=[b_ps[pb]])
            S.op("dve", lambda e, t=t, pb=pb: e.tensor_tensor(
                out=hT[:, :, 128 * t:128 * (t + 1)],
                in0=psb16(pb).rearrange("p (k t) -> p k t", k=8), in1=gmb, op=ALU.mult),
                reads=[b_ps[pb], b_gT], writes=[b_hT[t // 4]])

        evac_rr = [0]

        def evac(out_ap, in_ap, reads, writes):
            evac_rr[0] += 1
            if evac_rr[0] % 2 == 0:
                S.op("act", lambda e: e.copy(out=out_ap, in_=in_ap), reads=reads, writes=writes)
            else:
                S.op("dve", lambda e: e.tensor_copy(out=out_ap, in_=in_ap), reads=reads, writes=writes)

        pbank = [2]

        def proj_fm(wb, lhs_fn, dst, dbuf, tok_blocks):
            for (t0, n) in tok_blocks:
                pb = pbank[0]
                pbank[0] = 2 + (pbank[0] - 2 + 1) % 6
                for k in range(8):
                    S.op("pe", lambda e, k=k, pb=pb, t0=t0, n=n: e.matmul(
                        psb(pb)[:, 0:n], lhsT=lhs_fn(k), rhs=hT[:, k, t0:t0 + n], start=(k == 0), stop=(k == 7)),
                        reads=[wb, b_hT[t0 // 512]], writes=[b_ps[pb]])
                evac(dst[:, t0:t0 + n], psb(pb)[:, 0:n], [b_ps[pb]], [dbuf])

        tb_own = [(512 * i, 512) for i in range(4)]
        tb_all = [(512 * i, 512) for i in range(6)]
        tb_b = [(512 * i, 512) for i in range(4)] + [(2048, 128)]
        w0, w1 = wblk[0], wblk[1]
        for j in range(4):
            proj_fm(b_wblk[0], lambda k, j=j: w0[:, k, 128 * j:128 * (j + 1)], QT_A[:, j, :], b_QA, tb_own)
        for j in range(2):
            proj_fm(b_wblk[0], lambda k, j=j: w0[:, k, 512 + 128 * j:512 + 128 * (j + 1)], KT_A[:, j, :], b_KA, tb_all)
        load_wblk(2)
        for j in range(2):
            proj_fm(b_wblk[1], lambda k, j=j: w1[:, k, 128 * j:128 * (j + 1)], KT_A[:, 2 + j, :], b_KA, tb_all)
        for j in range(4):
            proj_fm(b_wblk[1], lambda k, j=j: w1[:, k, 256 + 128 * j:256 + 128 * (j + 1)], VT_A[:, j, :], b_VA, tb_all)
        for j in range(4):
            proj_fm(b_wblk[0], lambda k, j=j: w0[:, k, 128 * j:128 * (j + 1)], QT_B[:, j, :], b_QB, tb_own)
        for g in range(2):
            proj_fm(b_wkb, lambda k, g=g: wkb[:, k, g, :], KT_B[:, g, :], b_KB, tb_b)
        proj_fm(b_wblk[0], lambda k: w0[:, k, 640:768], VT_B, b_VB, tb_b)

        S.barrier()

        A.top = r3_off
        o_tm = A.alloc((16, 1024), BF16)
        after_o = A.top
        WA = A.alloc((24, 256), BF16)
        WB = A.alloc((8, 384), BF16)
        Vc = [A.alloc((32, 2, 65), BF16) for _ in range(2)]
        acc = [A.alloc((2048,), F32) for _ in range(2)]
        NEB = 4
        Eb = [A.alloc((512,), F32) for _ in range(NEB)]
        PT = [A.alloc((512,), BF16) for _ in range(NEB)]
        trel_i = A.alloc((128,), I32)
        trel = A.alloc((128,), F32)
        aabs = A.alloc((128,), F32)
        amask = A.alloc((128,), F32)
        etmp = [A.alloc((128,), F32) for _ in range(2)]
        rden = A.alloc((2, 16), F32)
        b_o = [Buf("o%d" % t) for t in range(16)]
        b_WA, b_WB = Buf("WA"), Buf("WB")
        b_Vc = [Buf("Vc0"), Buf("Vc1")]
        b_acc = [Buf("acc0"), Buf("acc1")]
        b_E = [Buf("E%d" % i) for i in range(NEB)]
        b_PT = [Buf("PT%d" % i) for i in range(NEB)]
        b_trel, b_aabs, b_amask = Buf("trel"), Buf("aabs"), Buf("amask")
        b_etmp = [Buf("etmp0"), Buf("etmp1")]
        b_rden = [Buf("rden0"), Buf("rden1")]

        S.op("pool", lambda e: e.iota(trel_i, pattern=[[1, 128]], base=0, channel_multiplier=-1), writes=[b_trel])
        S.op("dve", lambda e: e.tensor_copy(out=trel, in_=trel_i), reads=[b_trel], writes=[b_trel])
        S.op("pool", lambda e: e.memset(Vc[0][:, :, :, 64:65], 1.0), writes=[b_Vc[0]])
        S.op("pool", lambda e: e.memset(Vc[1][:, :, :, 64:65], 1.0), writes=[b_Vc[1]])
        wcount = [0]

        aab, amk, b_ab = {}, {}, {}
        for off, half in ((64, 64), (-64, 64), (128, 128), (0, 128), (-128, 128)):
            aab[off] = A.alloc((128,), F32)
            amk[off] = A.alloc((128,), F32)
            b_ab[off] = Buf("ab%d" % off)
            S.op("dve", lambda e, off=off: e.tensor_scalar(out=etmp[0], in0=trel, scalar1=float(off), scalar2=None,
                                                           op0=ALU.add),
                 reads=[b_trel], writes=[b_etmp[0]])
            S.op("dve", lambda e: e.tensor_scalar(out=etmp[1], in0=etmp[0], scalar1=-1.0, scalar2=None,
                                                  op0=ALU.mult),
                 reads=[b_etmp[0]], writes=[b_etmp[1]])
            S.op("dve", lambda e, off=off: e.tensor_max(out=aab[off], in0=etmp[0], in1=etmp[1]),
                 reads=[b_etmp[0], b_etmp[1]], writes=[b_ab[off]])
            S.op("dve", lambda e, off=off, half=half: e.tensor_single_scalar(
                out=amk[off], in_=aab[off], scalar=float(half) + 0.5, op=ALU.is_le),
                reads=[b_ab[off]], writes=[b_ab[off]])

        def gen_entries(off, entries):
            for (ns, out_ap, wbuf) in entries:
                i = wcount[0] % 2
                wcount[0] += 1
                S.op("act", lambda e, ns=ns, i=i: e.activation(out=etmp[i], in_=aab[off], func=AF.Exp,
                                                               scale=float(ns)),
                     reads=[b_ab[off]], writes=[b_etmp[i]])
                S.op("dve", lambda e, i=i, out_ap=out_ap: e.tensor_tensor(out=out_ap, in0=etmp[i], in1=amk[off],
                                                                          op=ALU.mult),
                     reads=[b_etmp[i], b_ab[off]], writes=[wbuf])

        CFG = ((128, 1), (512, 4), (2048, 16))
        slopes = [2.0 ** (-(h + 1)) for h in range(8)]
        b_WAp = [Buf("WAp%d" % i) for i in range(4)]

        def gen_pair_tables(jp):
            for part, off in ((0, 64), (1, -64)):
                ent = []
                for c, (_, d) in enumerate(CFG):
                    for h in (2 * jp, 2 * jp + 1):
                        ent.append((-slopes[h] * d, WA[:, c * 8 + h, 128 * part:128 * (part + 1)], b_WAp[jp]))
                gen_entries(off, ent)

        def gen_b_tables():
            for part, off in ((0, 128), (1, 0), (2, -128)):
                gen_entries(off, [(-slopes[h], WB[:, h, 128 * part:128 * (part + 1)], b_WB) for h in range(8)])

        gen_pair_tables(0)

        rot = {"st": 0, "e": 0, "pt": 0, "ot": 0}
        pending = []
        ST_BANKS = (0, 1, 2, 3)
        OT_BANKS = (4, 5)
        VTR_BANK = 6
        EP_BANKS = (6, 7, 6)
        SKEW = 2

        def submit(front, back):
            front()
            if len(pending) >= SKEW:
                pending.pop(0)()
            pending.append(back)

        def flush():
            while pending:
                for bk_ in pending.pop(0):
                    bk_()

        jobq = {0: [], 1: []}
        cur_q = [None]

        def run_job(blocks, actmul, segs, evac_fn, Kb, Qb, Vb, accb):
            st_ = {}

            def assign():
                st_["sb"] = ST_BANKS[rot["st"] % len(ST_BANKS)]
                rot["st"] += 1
                st_["ei"] = rot["e"] % NEB
                rot["e"] += 1
                st_["pi"] = rot["pt"] % NEB
                rot["pt"] += 1

            def qk():
                sb = st_["sb"]
                return [(lambda c0=c0, w=w, nk=nk, kt_ap=kt_ap, q_ap=q_ap: S.op(
                    "pe", lambda e: e.matmul(psb(sb)[0:nk, c0:c0 + w], lhsT=kt_ap, rhs=q_ap, start=True, stop=True),
                    reads=[Kb, Qb], writes=[b_ps[sb]])) for (c0, w, nk, kt_ap, q_ap) in blocks]

            def rest():
                sb, ei, pi = st_["sb"], st_["ei"], st_["pi"]
                for (rows, vf, w_ap, wbuf) in actmul:
                    S.op("act", lambda e, rows=rows, vf=vf: e.activation(
                        out=vf(Eb[ei])[0:rows], in_=vf(psb(sb))[0:rows], func=AF.Exp, scale=0.125),
                        reads=[b_ps[sb]], writes=[b_E[ei]])
                for (rows, vf, w_ap, wbuf) in actmul:
                    S.op("dve", lambda e, rows=rows, vf=vf, w_ap=w_ap: e.tensor_tensor(
                        out=vf(PT[pi])[0:rows], in0=vf(Eb[ei])[0:rows], in1=w_ap[0:rows], op=ALU.mult),
                        reads=[b_E[ei], wbuf], writes=[b_PT[pi]])

            def back():
                pi = st_["pi"]
                ob = OT_BANKS[rot["ot"] % 2]
                rot["ot"] += 1
                for (ocol, w, parts) in segs:
                    for n_i, (nk, lhsT_ap, pc0) in enumerate(parts):
                        S.op("pe", lambda e, ocol=ocol, w=w, nk=nk, lhsT_ap=lhsT_ap, pc0=pc0, n_i=n_i,
                             last=(n_i == len(parts) - 1): e.matmul(
                            psb(ob)[0:65, ocol:ocol + w], lhsT=lhsT_ap, rhs=PT[pi][0:nk, pc0:pc0 + w],
                            start=(n_i == 0), stop=last),
                            reads=[b_PT[pi], Vb], writes=[b_ps[ob]])
                out_ap, in_ap, is_add = evac_fn(psb(ob))
                if is_add:
                    S.op("dve", lambda e: e.tensor_tensor(out=out_ap, in0=out_ap, in1=in_ap, op=ALU.add),
                         reads=[b_ps[ob], accb], writes=[accb])
                else:
                    S.op("dve", lambda e: e.tensor_copy(out=out_ap, in_=in_ap), reads=[b_ps[ob]], writes=[accb])

            job = (assign, qk, rest, back)
            if cur_q[0] is None:
                emit_group([job])
            else:
                jobq[cur_q[0]].append(job)

        def emit_group(jobs):
            for j in jobs:
                j[0]()
            qks = [j[1]() for j in jobs]
            for i in range(max(len(q) for q in qks)):
                for q in qks:
                    if i < len(q):
                        q[i]()
            for j in jobs:
                j[2]()
            if len(pending) >= 1:
                for bk_ in pending.pop(0):
                    bk_()
            pending.append([j[3] for j in jobs])

        def emit_pairs():
            n0, n1 = len(jobq[0]), len(jobq[1])
            for i in range(max(n0, n1)):
                grp = []
                if i < n0:
                    grp.append(jobq[0][i])
                if i < n1:
                    grp.append(jobq[1][i])
                emit_group(grp)
            jobq[0], jobq[1] = [], []
            cur_q[0] = None

        def flat(v):
            return v

        def epilogue(acc_i, ch0, sink_h):
            a = acc[acc_i]
            groups = ((0, 7), (7, 7), (14, 2))
            for gi, (t0, n) in enumerate(groups):
                bk = EP_BANKS[gi]
                pv = psb(bk)[:, 0:455].rearrange("p (t c) -> p t c", c=65)
                for tt in range(n):
                    t = t0 + tt
                    S.op("pe", lambda e, pv=pv, tt=tt, t=t: e.transpose(
                        out=pv[:, tt, :], in_=a[0:65, 128 * t:128 * (t + 1)], identity=ident_f[0:65, 0:65]),
                        reads=[b_acc[acc_i], b_ident], writes=[b_ps[bk]])
                rd = rden[:, acc_i, t0:t0 + n]
                if sink_h is not None:
                    S.op("dve", lambda e, pv=pv, n=n, rd=rd: e.tensor_scalar(
                        out=rd, in0=pv[:, 0:n, 64], scalar1=esink[:, sink_h:sink_h + 1], scalar2=None, op0=ALU.add),
                        reads=[b_ps[bk], b_esink], writes=[b_rden[acc_i]])
                    S.op("dve", lambda e, rd=rd: e.reciprocal(out=rd, in_=rd), reads=[b_rden[acc_i]],
                         writes=[b_rden[acc_i]])
                else:
                    S.op("dve", lambda e, pv=pv, n=n, rd=rd: e.reciprocal(out=rd, in_=pv[:, 0:n, 64]),
                         reads=[b_ps[bk]], writes=[b_rden[acc_i]])
                S.op("dve", lambda e, pv=pv, n=n, rd=rd, t0=t0: e.tensor_tensor(
                    out=o_tm[:, t0:t0 + n, ch0:ch0 + 64], in0=pv[:, 0:n, 0:64],
                    in1=rd.unsqueeze(2).to_broadcast([128, n, 64]), op=ALU.mult),
                    reads=[b_ps[bk], b_rden[acc_i]], writes=[b_o[t] for t in range(t0, t0 + n)])

        vrot = [0]

        def build_v(src_fn, tiles, vbuf_i, nheads_cols):
            vb = Vc[vbuf_i]
            i = 0
            while i < len(tiles):
                grp = tiles[i:i + 8]
                for s_, (tidx, nk, src) in enumerate(grp):
                    S.op("pe", lambda e, s_=s_, nk=nk, src=src: e.transpose(
                        out=psb16(VTR_BANK)[0:nk, 128 * s_:128 * (s_ + 1)], in_=src, identity=ident_b),
                        reads=[src_fn, b_ident], writes=[b_ps[VTR_BANK]])
                t0 = grp[0][0]
                n = len(grp)
                S.op("act", lambda e, t0=t0, n=n: e.copy(
                    out=vb[:, t0:t0 + n, :, 0:64],
                    in_=psb16(VTR_BANK)[:, 0:128 * n].rearrange("p (t h c) -> p t h c", t=n, h=2)),
                    reads=[b_ps[VTR_BANK]], writes=[b_Vc[vbuf_i]])
                i += 8

        def dil(ap2, d):
            return ap2.rearrange("p (u d) -> p d u", d=d)

        def e3(n, w):
            return lambda tl: tl[:, 0:n * w].rearrange("p (n w) -> p n w", n=n)

        vsel = [0]
        if stage >= 2:
            for jj in range(4):
                if jj + 1 < 4:
                    gen_pair_tables(jj + 1)
                if jj == 2:
                    gen_b_tables()
                b_WA = b_WAp[jj]
                for c, (_, d) in enumerate(CFG):
                    N = NTOK // d // 128
                    nt = N + 1
                    vi = vsel[0] % 2
                    vsel[0] += 1
                    vsrc = dil(VT_A[:, jj, :], d)
                    tiles = []
                    for r in range(d):
                        for j in range(nt):
                            nk = 64 if (d == 16 and j == 1) else 128
                            tiles.append((r * nt + j, nk, vsrc[:, r, 128 * j:128 * j + nk]))
                    build_v(b_VA, tiles, vi, 2)
                    vb = Vc[vi]
                    for e_ in range(2):
                        cur_q[0] = e_
                        h = 2 * jj + e_
                        pr = slice(64 * e_, 64 * e_ + 64)
                        qd = dil(QT_A[pr, jj, :], d)
                        kd = dil(KT_A[pr, jj, :], d)
                        accv = dil(acc[e_][0:65, :], d)
                        hc = c * 8 + h
                        Wfull = WA[:, hc, :]
                        is_add = (c > 0)
                        nkL = 64 if d == 16 else 128

                        def ktile(r, j, nk=128):
                            return kd[:, r, 128 * j:128 * j + nk]

                        def vt(r, j, nk=128):
                            return vb[0:nk, r * nt + j, e_, :]

                        for r0 in range(0, d, 8):
                            n = min(8, d - r0)
                            blocks, segs = [], []
                            for rr in range(n):
                                r = r0 + rr
                                blocks.append((64 * rr, 64, 128, ktile(r, 0), qd[:, r, 0:64]))
                                segs.append((64 * rr, 64, [(128, vt(r, 0), 64 * rr)]))
                            am = [(128, e3(n, 64), Wfull[:, 192:256].unsqueeze(1).to_broadcast([128, n, 64]), b_WA)]
                            run_job(blocks, am, segs,
                                    lambda ot, r0=r0, n=n, accv=accv, is_add=is_add: (
                                        accv[:, r0:r0 + n, 0:64], e3(n, 64)(ot)[0:65], is_add),
                                    b_KA, b_QA, b_Vc[vi], b_acc[e_])
                        full = list(range(1, N))
                        for r in range(d):
                            ii = 0
                            while ii + 1 < len(full):
                                i0 = full[ii]
                                blocks, segs = [], []
                                for sgi in range(2):
                                    i_ = i0 + sgi
                                    q_ap = qd[:, r, 128 * i_ - 64:128 * i_ + 64]
                                    blocks.append((256 * sgi, 128, 128, ktile(r, i_ - 1), q_ap))
                                    blocks.append((256 * sgi + 128, 128, 128, ktile(r, i_), q_ap))
                                    segs.append((128 * sgi, 128, [(128, vt(r, i_ - 1), 256 * sgi),
                                                                  (128, vt(r, i_), 256 * sgi + 128)]))
                                am = [(128, e3(2, 256), Wfull.unsqueeze(1).to_broadcast([128, 2, 256]), b_WA)]
                                run_job(blocks, am, segs,
                                        lambda ot, r=r, i0=i0, accv=accv, is_add=is_add: (
                                            accv[:, r:r + 1, 128 * i0 - 64:128 * i0 + 192], e3(1, 256)(ot)[0:65],
                                            is_add),
                                        b_KA, b_QA, b_Vc[vi], b_acc[e_])
                                ii += 2
                        if len(full) % 2 == 1:
                            i_ = full[-1]
                            for r0 in range(0, d, 2):
                                n = min(2, d - r0)
                                blocks, segs = [], []
                                for rr in range(n):
                                    r = r0 + rr
                                    q_ap = qd[:, r, 128 * i_ - 64:128 * i_ + 64]
                                    blocks.append((256 * rr, 128, 128, ktile(r, i_ - 1), q_ap))
                                    blocks.append((256 * rr + 128, 128, 128, ktile(r, i_), q_ap))
                                    segs.append((128 * rr, 128, [(128, vt(r, i_ - 1), 256 * rr),
                                                                 (128, vt(r, i_), 256 * rr + 128)]))
                                am = [(128, e3(n, 256), Wfull.unsqueeze(1).to_broadcast([128, n, 256]), b_WA)]
                                run_job(blocks, am, segs,
                                        lambda ot, r0=r0, n=n, i_=i_, accv=accv, is_add=is_add: (
                                            accv[:, r0:r0 + n, 128 * i_ - 64:128 * i_ + 64], e3(n, 128)(ot)[0:65],
                                            is_add),
                                        b_KA, b_QA, b_Vc[vi], b_acc[e_])
                        for r0 in range(0, d, 4):
                            n = min(4, d - r0)
                            blocks, segs = [], []
                            for rr in range(n):
                                r = r0 + rr
                                q_ap = qd[:, r, 128 * N - 64:128 * N]
                                blocks.append((128 * rr, 64, 128, ktile(r, N - 1), q_ap))
                                blocks.append((128 * rr + 64, 64, nkL, ktile(r, N, nkL), q_ap))
                                segs.append((64 * rr, 64, [(128, vt(r, N - 1), 128 * rr),
                                                            (nkL, vt(r, N, nkL), 128 * rr + 64)]))

                            def vprev(tl, n=n):
                                return tl[:, 0:128 * n].rearrange("p (n a w) -> p n a w", n=n, a=2)[:, :, 0, :]

                            def vcur(tl, n=n):
                                return tl[:, 0:128 * n].rearrange("p (n a w) -> p n a w", n=n, a=2)[:, :, 1, :]

                            am = [(128, vprev, Wfull[:, 0:64].unsqueeze(1).to_broadcast([128, n, 64]), b_WA),
                                  (nkL, vcur, Wfull[:, 128:192].unsqueeze(1).to_broadcast([128, n, 64]), b_WA)]
                            run_job(blocks, am, segs,
                                    lambda ot, r0=r0, n=n, N=N, accv=accv, is_add=is_add: (
                                        accv[:, r0:r0 + n, 128 * N - 64:128 * N], e3(n, 64)(ot)[0:65], is_add),
                                    b_KA, b_QA, b_Vc[vi], b_acc[e_])
                    emit_pairs()
                flush()
                for e_ in range(2):
                    epilogue(e_, 64 * (2 * jj + e_), None)
                if jj == 3:
                    for t in range(16):
                        S.op("act", lambda e, t=t: e.activation(
                            out=junk3, in_=o_tm[:, t, 0:512], func=AF.Square, accum_out=st3[:, 0, t:t + 1]),
                            reads=[b_o[t]], writes=[b_junk3, b_st3])

            vi = vsel[0] % 2
            vsel[0] += 1
            tiles = [(j, 128, VT_B[:, 128 * j:128 * (j + 1)]) for j in range(17)]
            build_v(b_VB, tiles, vi, 2)
            vb = Vc[vi]
            for h in range(8):
                jj, e_, g = h // 2, h % 2, h // 4
                cur_q[0] = e_
                pr = slice(64 * e_, 64 * e_ + 64)
                ai = h % 2
                for i_ in range(16):
                    jlo, jhi = max(0, i_ - 1), min(16, i_ + 1)
                    nb = jhi - jlo + 1
                    q_ap = QT_B[pr, jj, 128 * i_:128 * (i_ + 1)]
                    blocks, parts = [], []
                    for bi, j in enumerate(range(jlo, jhi + 1)):
                        blocks.append((128 * bi, 128, 128, KT_B[pr, g, 128 * j:128 * (j + 1)], q_ap))
                        parts.append((128, vb[:, j, g, :], 128 * bi))
                    wo = 128 * (jlo - i_ + 1)
                    am = [(128, (lambda tl, nb=nb: tl[:, 0:128 * nb]), WB[:, h, wo:wo + 128 * nb], b_WB)]
                    run_job(blocks, am, [(0, 128, parts)],
                            lambda ot, i_=i_, ai=ai: (acc[ai][0:65, 128 * i_:128 * (i_ + 1)], ot[0:65, 0:128], False),
                            b_KB, b_QB, b_Vc[vi], b_acc[ai])
                if e_ == 1:
                    emit_pairs()
                    flush()
                    epilogue(0, 512 + 64 * (h - 1), h - 1)
                    epilogue(1, 512 + 64 * h, h)

        S.barrier()

        def pipeline(n, stages):
            ns = len(stages)
            for i in range(n + ns - 1):
                for s_, f_ in enumerate(stages):
                    t_ = i - s_
                    if 0 <= t_ < n:
                        f_(t_)

        A.top = r2_off
        wout_b = A.alloc((8, 1024), BF16)
        xs3 = [A.alloc((1024,), F32) for _ in range(3)]
        assert A.top <= r3_off
        A.top = after_o
        mixn = [A.alloc((1024,), BF16) for _ in range(3)]
        mixT = [A.alloc((8, 128), BF16) for _ in range(2)]
        b_wout = Buf("wout")
        b_xs3 = [Buf("xs3_%d" % i) for i in range(3)]
        b_mixn = [Buf("mixn%d" % i) for i in range(3)]
        b_mixT = [Buf("mixT0"), Buf("mixT1")]
        b_x1 = [[Buf("x1_%d_%d" % (t, hf)) for hf in range(2)] for t in range(16)]

        S.dma("pool", lambda e: e.dma_start(out=wout_b, in_=w_out.rearrange("(k p) c -> p k c", p=128)),
              writes=[b_wout])
        ggb = gT[:, 24:32].unsqueeze(2).to_broadcast([128, 8, 128])
        if stage >= 2:
            for t in range(16):
                for gi in range(1, 2):
                    S.op("act", lambda e, t=t, gi=gi: e.activation(
                        out=junk3, in_=o_tm[:, t, 512 * gi:512 * (gi + 1)], func=AF.Square,
                        accum_out=st3[:, 0, 16 * gi + t:16 * gi + t + 1]),
                        reads=[b_o[t]], writes=[b_junk3, b_st3])
            rstd_ops(st3[:, 0, :], st3[:, 1, :], st3[:, 2, :], 512, [b_st3])

            def a3_s0(t):
                s = t % 3
                S.dma("sp", lambda e: e.dma_start(out=xs3[s], in_=xl[128 * t:128 * (t + 1), :]), writes=[b_xs3[s]])
                for gi in range(2):
                    S.op("dve", lambda e, gi=gi: e.tensor_scalar(
                        out=mixn[s][:, 512 * gi:512 * (gi + 1)], in0=o_tm[:, t, 512 * gi:512 * (gi + 1)],
                        scalar1=st3[:, 2, 16 * gi + t:16 * gi + t + 1], scalar2=None, op0=ALU.mult),
                        reads=[b_o[t], b_st3], writes=[b_mixn[s]])
                pb = t % 2
                for k in range(8):
                    S.op("pe", lambda e, k=k: e.transpose(
                        out=psb16(pb)[:, 128 * k:128 * (k + 1)], in_=mixn[s][:, 128 * k:128 * (k + 1)],
                        identity=ident_b), reads=[b_mixn[s], b_ident], writes=[b_ps[pb]])

            def a3_s1(t):
                pb = t % 2
                m = t % 2
                S.op("dve", lambda e: e.tensor_tensor(
                    out=mixT[m], in0=psb16(pb).rearrange("p (k t) -> p k t", k=8), in1=ggb, op=ALU.mult),
                    reads=[b_ps[pb], b_gT], writes=[b_mixT[m]])
                for hf in range(2):
                    ob = 2 + (2 * t + hf) % 4
                    for k in range(8):
                        S.op("pe", lambda e, k=k, hf=hf, ob=ob: e.matmul(
                            psb(ob), lhsT=mixT[m][:, k, :], rhs=wout_b[:, k, 512 * hf:512 * (hf + 1)],
                            start=(k == 0), stop=(k == 7)),
                            reads=[b_mixT[m], b_wout], writes=[b_ps[ob]])

            def a3_s2(t):
                s = t % 3
                for hf in range(2):
                    ob = 2 + (2 * t + hf) % 4
                    S.op("dve", lambda e, hf=hf, ob=ob: e.tensor_tensor(
                        out=x1[:, t, 512 * hf:512 * (hf + 1)], in0=xs3[s][:, 512 * hf:512 * (hf + 1)],
                        in1=psb(ob), op=ALU.add),
                        reads=[b_xs3[s], b_ps[ob]], writes=[b_x1[t][hf]])
                if stage >= 3:
                    S.op("act", lambda e: e.activation(out=junk4, in_=x1[:, t, :], func=AF.Square,
                                                       accum_out=stm[:, 0, t:t + 1]),
                         reads=b_x1[t], writes=[b_junk4, b_stm])

            pipeline(16, [a3_s0, a3_s1, a3_s2])
        else:
            for t in range(16):
                s = t % 3
                S.dma("sp", lambda e, t=t, s=s: e.dma_start(out=xs3[s], in_=xl[128 * t:128 * (t + 1), :]),
                      writes=[b_xs3[s]])
                for hf in range(2):
                    S.op("dve", lambda e, t=t, s=s, hf=hf: e.tensor_copy(
                        out=x1[:, t, 512 * hf:512 * (hf + 1)], in_=xs3[s][:, 512 * hf:512 * (hf + 1)]),
                        reads=[b_xs3[s]], writes=[b_x1[t][hf]])

        S.barrier()

        def dump_x1():
            for t in range(16):
                S.dma("sp", lambda e, t=t: e.dma_start(out=dbg_o[128 * t:128 * (t + 1), :], in_=x1[:, t, :]),
                      reads=b_x1[t], sem="d_dbg")

        if dbg and stage <= 2:
            dump_x1()

        A.top = r2_off
        hT2 = A.alloc((8, NTOK), BF16)
        NSLOT = 3
        wg_b = [A.alloc((8, 512), BF16) for _ in range(2)]
        wu_b = [A.alloc((8, 512), BF16) for _ in range(2)]
        wd_b = [A.alloc((4, 1024), BF16) for _ in range(2)]
        sg = [A.alloc((512,), F32) for _ in range(2)]
        hid = [A.alloc((4, 512), BF16) for _ in range(2)]
        m1_mark = A.top
        xnf = [A.alloc((1024,), F32) for _ in range(2)]
        h2f = [A.alloc((8, 128), F32) for _ in range(2)]
        junkm = A.alloc((1024,), BF16)
        rt = A.alloc((16 * 136,), F32)
        b_hT2 = [Buf("hT2_%d" % i) for i in range(4)]
        b_wg = [Buf("wg%d" % i) for i in range(NSLOT)]
        b_wu = [Buf("wu%d" % i) for i in range(NSLOT)]
        b_wd = [Buf("wd%d" % i) for i in range(NSLOT)]
        b_sg = [Buf("sg0"), Buf("sg1")]
        b_hid = [Buf("hid0"), Buf("hid1")]
        b_xnf = [Buf("xnf0"), Buf("xnf1")]
        b_h2f = [Buf("h2f0"), Buf("h2f1")]
        b_junkm = Buf("junkm")
        b_rt = Buf("rt")

        def load_expert(ei):
            sl = ei % NSLOT
            S.dma("pool", lambda e: e.dma_start(out=wg_b[sl], in_=w_eg[ei].rearrange("(k p) f -> p k f", p=128)),
                  writes=[b_wg[sl]])
            S.dma("pool", lambda e: e.dma_start(out=wu_b[sl], in_=w_eu[ei].rearrange("(k p) f -> p k f", p=128)),
                  writes=[b_wu[sl]])
            S.dma("pool", lambda e: e.dma_start(out=wd_b[sl], in_=w_ed[ei].rearrange("(k p) f -> p k f", p=128)),
                  writes=[b_wd[sl]])

        NEXP = 32 if stage >= 3 else 0
        for ei in range(min(2, NEXP)):
            load_expert(ei)

        if stage >= 3:
            rstd_ops(stm[:, 0, :], stm[:, 1, :], stm[:, 2, :], 1024, [b_stm])

            def m1_s0(t):
                s = t % 2
                S.op("dve", lambda e: e.tensor_scalar(out=xnf[s], in0=x1[:, t, :], scalar1=stm[:, 2, t:t + 1],
                                                      scalar2=None, op0=ALU.mult),
                     reads=b_x1[t] + [b_stm], writes=[b_xnf[s]])
                pb0 = 2 * (t % 2)
                for k in range(8):
                    bk = pb0 + k // 4
                    S.op("pe", lambda e, k=k, bk=bk: e.transpose(
                        out=psb(bk)[:, 128 * (k % 4):128 * (k % 4 + 1)], in_=xnf[s][:, 128 * k:128 * (k + 1)],
                        identity=ident_f), reads=[b_xnf[s], b_ident], writes=[b_ps[bk]])

            def m1_s1(t):
                s = t % 2
                pb0 = 2 * (t % 2)
                for hh in range(2):
                    bk = pb0 + hh
                    S.op("dve", lambda e, bk=bk, hh=hh: e.tensor_tensor(
                        out=h2f[s][:, 4 * hh:4 * hh + 4, :], in0=psb(bk).rearrange("p (k t) -> p k t", k=4),
                        in1=gT[:, 8 + 4 * hh:12 + 4 * hh].unsqueeze(2).to_broadcast([128, 4, 128]), op=ALU.mult),
                        reads=[b_ps[bk], b_gT], writes=[b_h2f[s]])
                S.op("pool", lambda e: e.tensor_copy(out=hT2[:, :, 128 * t:128 * (t + 1)], in_=h2f[s]),
                     reads=[b_h2f[s]], writes=[b_hT2[t // 4]])
                lb = 4 + t // 8
                c0 = 36 * (t % 8)
                for k in range(8):
                    S.op("pe", lambda e, k=k: e.matmul(
                        psb(lb)[:, c0:c0 + 36], lhsT=h2f[s][:, k, :], rhs=wr[:, k, :], start=(k == 0), stop=(k == 7)),
                        reads=[b_h2f[s], b_wr], writes=[b_ps[lb]])

            pipeline(16, [m1_s0, m1_s1])

            lg = rt[:, 0:576].rearrange("p (t c) -> p t c", t=16)
            gl = lg[:, :, 0:4]
            el = lg[:, :, 4:36].rearrange("p t (g e) -> p t g e", g=4)
            o_ = [576]

            def carve(n):
                v = rt[:, o_[0]:o_[0] + n]
                o_[0] += n
                return v

            gmax, gsum, gw, m1v, m2v, dm, ex, den_, w1, w2 = [carve(16) for _ in range(10)]
            goh = carve(64).rearrange("p (t g) -> p t g", t=16)
            gsh = carve(64).rearrange("p (t g) -> p t g", t=16)
            tmp = carve(512).rearrange("p (t g e) -> p t g e", t=16, g=4)
            esel = carve(128).rearrange("p (t e) -> p t e", t=16)
            oh1 = carve(128).rearrange("p (t e) -> p t e", t=16)
            e2 = carve(128).rearrange("p (t e) -> p t e", t=16)
            oh2 = carve(128).rearrange("p (t e) -> p t e", t=16)
            we = carve(128).rearrange("p (t e) -> p t e", t=16)
            we2 = carve(128).rearrange("p (t e) -> p t e", t=16)
            assert o_[0] <= 16 * 136
            rb = [b_rt]

            def D(fn, extra_r=(), extra_w=()):
                S.op("dve", fn, reads=rb + list(extra_r), writes=rb + list(extra_w))

            def bc3(v, n):
                return v.unsqueeze(2).to_broadcast([128, 16, n])

            for hb in range(2):
                D(lambda e, hb=hb: e.tensor_tensor(
                    out=lg[:, 8 * hb:8 * hb + 8, :], in0=psb(4 + hb)[:, 0:288].rearrange("p (t c) -> p t c", t=8),
                    in1=rbias.unsqueeze(1).to_broadcast([128, 8, 36]), op=ALU.add), [b_ps[4 + hb], b_rbias])
            D(lambda e: e.reduce_max(out=gmax, in_=gl, axis=AX.X))
            D(lambda e: e.tensor_tensor(out=goh, in0=gl, in1=bc3(gmax, 4), op=ALU.is_equal))
            D(lambda e: e.tensor_tensor(out=gsh, in0=gl, in1=bc3(gmax, 4), op=ALU.subtract))
            S.op("act", lambda e: e.activation(out=gsh, in_=gsh, func=AF.Exp), reads=rb, writes=rb)
            D(lambda e: e.reduce_sum(out=gsum, in_=gsh, axis=AX.X))
            D(lambda e: e.reciprocal(out=gw, in_=gsum))
            D(lambda e: e.tensor_tensor(out=tmp, in0=el, in1=goh.unsqueeze(3).to_broadcast([128, 16, 4, 8]),
                                        op=ALU.mult))
            D(lambda e: e.tensor_reduce(out=esel, in_=tmp.rearrange("p t g e -> p t e g"), axis=AX.X, op=ALU.add))
            D(lambda e: e.reduce_max(out=m1v, in_=esel, axis=AX.X))
            D(lambda e: e.tensor_tensor(out=oh1, in0=esel, in1=bc3(m1v, 8), op=ALU.is_equal))
            D(lambda e: e.scalar_tensor_tensor(out=e2, in0=oh1, scalar=-1e30, in1=esel, op0=ALU.mult, op1=ALU.add))
            D(lambda e: e.reduce_max(out=m2v, in_=e2, axis=AX.X))
            D(lambda e: e.tensor_tensor(out=oh2, in0=e2, in1=bc3(m2v, 8), op=ALU.is_equal))
            D(lambda e: e.tensor_tensor(out=dm, in0=m2v, in1=m1v, op=ALU.subtract))
            S.op("act", lambda e: e.activation(out=ex, in_=dm, func=AF.Exp), reads=rb, writes=rb)
            D(lambda e: e.tensor_scalar(out=den_, in0=ex, scalar1=1.0, scalar2=None, op0=ALU.add))
            D(lambda e: e.reciprocal(out=den_, in_=den_))
            D(lambda e: e.tensor_tensor(out=w1, in0=den_, in1=gw, op=ALU.mult))
            D(lambda e: e.tensor_tensor(out=w2, in0=w1, in1=ex, op=ALU.mult))
            D(lambda e: e.tensor_tensor(out=we, in0=oh1, in1=bc3(w1, 8), op=ALU.mult))
            D(lambda e: e.tensor_tensor(out=we2, in0=oh2, in1=bc3(w2, 8), op=ALU.mult))
            D(lambda e: e.tensor_tensor(out=we, in0=we, in1=we2, op=ALU.add))
            D(lambda e: e.tensor_tensor(
                out=dw.rearrange("p t (g e) -> p t g e", g=4),
                in0=goh.unsqueeze(3).to_broadcast([128, 16, 4, 8]),
                in1=we.unsqueeze(2).to_broadcast([128, 16, 4, 8]), op=ALU.mult), (), b_dw)

        S.barrier()
        A.top = m1_mark
        wg_b.append(A.alloc((8, 512), BF16))
        wu_b.append(A.alloc((8, 512), BF16))
        wd_b.append(A.alloc((4, 1024), BF16))
        ucount = [0]
        wpg_b = A.alloc((8, 1024), BF16)
        b_wpg = Buf("wpg")

        def p_stats(tb):
            if stage >= 4:
                for t in range(4 * tb, 4 * tb + 4):
                    S.op("act", lambda e, t=t: e.activation(out=junk4, in_=x1[:, t, :], func=AF.Square,
                                                            accum_out=st4[:, 0, t:t + 1]),
                         reads=b_x1[t], writes=[b_junk4, b_st4])
        GB = (0, 1)
        UB = (2, 3)
        DB = (4, 5, 6, 7)
        grot = [0]
        drot = [0]
        mpend = [None]

        def expert_unit(ei, tb):
            sl = ei % NSLOT
            hi = ucount[0] % 2
            ucount[0] += 1

            def front():
                for f in range(4):
                    gi = grot[0] % 2
                    grot[0] += 1
                    gb, ub = GB[gi], UB[gi]
                    for k in range(8):
                        S.op("pe", lambda e, k=k, f=f, gb=gb: e.matmul(
                            psb(gb), lhsT=wg_b[sl][:, k, 128 * f:128 * (f + 1)],
                            rhs=hT2[:, k, 512 * tb:512 * (tb + 1)], start=(k == 0), stop=(k == 7)),
                            reads=[b_wg[sl], b_hT2[tb]], writes=[b_ps[gb]])
                    for k in range(8):
                        S.op("pe", lambda e, k=k, f=f, ub=ub: e.matmul(
                            psb(ub), lhsT=wu_b[sl][:, k, 128 * f:128 * (f + 1)],
                            rhs=hT2[:, k, 512 * tb:512 * (tb + 1)], start=(k == 0), stop=(k == 7)),
                            reads=[b_wu[sl], b_hT2[tb]], writes=[b_ps[ub]])
                    S.op("act", lambda e, gb=gb, gi=gi: e.activation(out=sg[gi], in_=psb(gb), func=AF.Silu),
                         reads=[b_ps[gb]], writes=[b_sg[gi]])
                    S.op("dve", lambda e, ub=ub, gi=gi, f=f: e.tensor_tensor(
                        out=hid[hi][:, f, :], in0=sg[gi], in1=psb(ub), op=ALU.mult),
                        reads=[b_sg[gi], b_ps[ub]], writes=[b_hid[hi]])

            def back():
                for tt in range(4):
                    t = 4 * tb + tt
                    for hf in range(2):
                        db = DB[drot[0] % 4]
                        drot[0] += 1
                        for fk in range(4):
                            S.op("pe", lambda e, fk=fk, tt=tt, hf=hf, db=db: e.matmul(
                                psb(db), lhsT=hid[hi][:, fk, 128 * tt:128 * (tt + 1)],
                                rhs=wd_b[sl][:, fk, 512 * hf:512 * (hf + 1)], start=(fk == 0), stop=(fk == 3)),
                                reads=[b_hid[hi], b_wd[sl]], writes=[b_ps[db]])
                        S.op("dve", lambda e, t=t, hf=hf, db=db: e.scalar_tensor_tensor(
                            out=x1[:, t, 512 * hf:512 * (hf + 1)], in0=psb(db), scalar=dw[:, t, ei:ei + 1],
                            in1=x1[:, t, 512 * hf:512 * (hf + 1)], op0=ALU.mult, op1=ALU.add),
                            reads=[b_ps[db], b_dw[t], b_x1[t][hf]], writes=[b_x1[t][hf]])

            front()
            if mpend[0] is not None:
                mpend[0]()
            mpend[0] = back

        for ei in range(NEXP):
            for tb in range(4):
                expert_unit(ei, tb)
                if tb == 0 and ei + NSLOT - 1 < NEXP:
                    load_expert(ei + NSLOT - 1)
                if tb == 0 and ei == NEXP - 2:
                    S.dma("pool", lambda e: e.dma_start(out=wpg_b, in_=w_pg.rearrange("(k p) c -> p k c", p=128)),
                          writes=[b_wpg])
                if ei == NEXP - 1 and tb >= 1:
                    p_stats(tb - 1)
        if mpend[0] is not None:
            mpend[0]()
            mpend[0] = None
        if NEXP > 0:
            p_stats(3)

        S.barrier()
        if dbg and stage == 3:
            dump_x1()

        A.top = r2_off
        gfin = A.alloc((1024,), F32)
        S.dma("sp", lambda e: e.dma_start(out=gfin, in_=g_final.partition_broadcast(128)), writes=[b_gfin])
        wpp_b = A.alloc((2, 1024), BF16)
        pbf = [A.alloc((256,), BF16) for _ in range(2)]
        pT = [A.alloc((2, 128), BF16) for _ in range(2)]
        xn4 = [A.alloc((1024,), BF16) for _ in range(2)]
        hT3 = [A.alloc((8, 128), BF16) for _ in range(2)]
        sgm = [A.alloc((512,), F32) for _ in range(2)]
        yt = [A.alloc((1024,), F32) for _ in range(3)]
        b_wpp = Buf("wpp")
        b_pbf = [Buf("pbf0"), Buf("pbf1")]
        b_pT = [Buf("pT0"), Buf("pT1")]
        b_xn4 = [Buf("xn4_0"), Buf("xn4_1")]
        b_hT3 = [Buf("hT3_0"), Buf("hT3_1")]
        b_sgm = [Buf("sgm0"), Buf("sgm1")]
        b_yt = [Buf("yt%d" % i) for i in range(3)]
        b_st4f = [Buf("st4f%d" % g) for g in range(4)]

        if NEXP < 2:
            S.dma("pool", lambda e: e.dma_start(out=wpg_b, in_=w_pg.rearrange("(k p) c -> p k c", p=128)),
                  writes=[b_wpg])
        S.dma("pool", lambda e: e.dma_start(out=wpp_b, in_=w_pp.rearrange("(k p) c -> p k c", p=128)),
              writes=[b_wpp])
        gpb = gT[:, 16:24].unsqueeze(2).to_broadcast([128, 8, 128])
        ycount = [0]

        def final_group(g):
            ts_ = list(range(4 * g, 4 * g + 4))
            for t in ts_:
                S.op("act", lambda e, t=t: e.activation(out=junk4, in_=x1[:, t, :], func=AF.Square,
                                                        accum_out=st4[:, 3, t:t + 1]),
                     reads=b_x1[t], writes=[b_junk4, b_st4f[g]])
            sl_ = slice(4 * g, 4 * g + 4)
            rstd_ops(st4[:, 3, sl_], st4[:, 4, sl_], st4[:, 5, sl_], 1024, [b_st4f[g]])
            for t in ts_:
                s = ycount[0] % 3
                ycount[0] += 1
                S.op("dve", lambda e, t=t, s=s: e.scalar_tensor_tensor(
                    out=yt[s], in0=x1[:, t, :], scalar=st4[:, 5, t:t + 1], in1=gfin, op0=ALU.mult, op1=ALU.mult),
                    reads=b_x1[t] + [b_st4f[g], b_gfin], writes=[b_yt[s]])
                S.dma("sp", lambda e, t=t, s=s: e.dma_start(out=y[128 * t:128 * (t + 1), :], in_=yt[s]),
                      reads=[b_yt[s]], sem="d_y%d" % s)

        if stage >= 4:
            if NEXP == 0:
                for t in range(16):
                    S.op("act", lambda e, t=t: e.activation(out=junk4, in_=x1[:, t, :], func=AF.Square,
                                                            accum_out=st4[:, 0, t:t + 1]),
                         reads=b_x1[t], writes=[b_junk4, b_st4])
            rstd_ops(st4[:, 0, :], st4[:, 1, :], st4[:, 2, :], 1024, [b_st4])
            ppend = [None]

            def p_s0(t):
                s = t % 2
                S.dma("pool", lambda e: e.dma_start(out=pbf[s], in_=pl[128 * t:128 * (t + 1), :]), writes=[b_pbf[s]])
                S.op("dve", lambda e: e.tensor_scalar(out=xn4[s], in0=x1[:, t, :], scalar1=st4[:, 2, t:t + 1],
                                                      scalar2=None, op0=ALU.mult),
                     reads=b_x1[t] + [b_st4], writes=[b_xn4[s]])
                pb = t % 2
                for k in range(8):
                    S.op("pe", lambda e, k=k: e.transpose(
                        out=psb16(pb)[:, 128 * k:128 * (k + 1)], in_=xn4[s][:, 128 * k:128 * (k + 1)],
                        identity=ident_b), reads=[b_xn4[s], b_ident], writes=[b_ps[pb]])
                tp = 2 + t % 2
                for c in range(2):
                    S.op("pe", lambda e, c=c: e.transpose(
                        out=psb16(tp)[:, 128 * c:128 * (c + 1)], in_=pbf[s][:, 128 * c:128 * (c + 1)],
                        identity=ident_b), reads=[b_pbf[s], b_ident], writes=[b_ps[tp]])

            def p_s1(t):
                s = t % 2
                pb = t % 2
                tp = 2 + t % 2
                S.op("dve", lambda e: e.tensor_tensor(
                    out=hT3[s], in0=psb16(pb).rearrange("p (k t) -> p k t", k=8), in1=gpb, op=ALU.mult),
                    reads=[b_ps[pb], b_gT], writes=[b_hT3[s]])
                S.op("act", lambda e: e.copy(
                    out=pT[s], in_=psb16(tp)[:, 0:256].rearrange("p (c t) -> p c t", c=2)),
                    reads=[b_ps[tp]], writes=[b_pT[s]])
                for hf in range(2):
                    u = 2 * t + hf
                    gbk = 4 + 2 * (u % 2)
                    pbk = 5 + 2 * (u % 2)
                    for k in range(8):
                        S.op("pe", lambda e, k=k, hf=hf, gbk=gbk: e.matmul(
                            psb(gbk), lhsT=hT3[s][:, k, :], rhs=wpg_b[:, k, 512 * hf:512 * (hf + 1)],
                            start=(k == 0), stop=(k == 7)),
                            reads=[b_hT3[s], b_wpg], writes=[b_ps[gbk]])
                    for c in range(2):
                        S.op("pe", lambda e, c=c, hf=hf, pbk=pbk: e.matmul(
                            psb(pbk), lhsT=pT[s][:, c, :], rhs=wpp_b[:, c, 512 * hf:512 * (hf + 1)],
                            start=(c == 0), stop=(c == 1)),
                            reads=[b_pT[s], b_wpp], writes=[b_ps[pbk]])

                    def tail(t=t, hf=hf, u=u, gbk=gbk, pbk=pbk):
                        sm = u % 2
                        S.op("act", lambda e: e.activation(out=sgm[sm], in_=psb(gbk), func=AF.Sigmoid),
                             reads=[b_ps[gbk]], writes=[b_sgm[sm]])
                        S.op("dve", lambda e: e.tensor_tensor(out=sgm[sm], in0=sgm[sm], in1=psb(pbk), op=ALU.mult),
                             reads=[b_sgm[sm], b_ps[pbk]], writes=[b_sgm[sm]])
                        S.op("dve", lambda e: e.tensor_tensor(
                            out=x1[:, t, 512 * hf:512 * (hf + 1)], in0=x1[:, t, 512 * hf:512 * (hf + 1)],
                            in1=sgm[sm], op=ALU.add), reads=[b_sgm[sm], b_x1[t][hf]], writes=[b_x1[t][hf]])
                        if hf == 1 and t % 4 == 3:
                            final_group(t // 4)

                    if ppend[0] is not None:
                        ppend[0]()
                    ppend[0] = tail

            pipeline(16, [p_s0, p_s1])
            if ppend[0] is not None:
                ppend[0]()
                ppend[0] = None
        else:
            for g in range(4):
                final_group(g)

        S.barrier()
        S.emit()
    return nc


_NC_CACHE = {}


def _get_nc(stage, dbg):
    key = (stage, dbg)
    if key not in _NC_CACHE:
        _NC_CACHE[key] = build_nc(stage, dbg)
    return _NC_CACHE[key]


def kernel(x, p, g_mix, w_in, sink, g_grp_a, g_grp_b, w_out, g_ffn,
           w_router_group, b_router_group, w_router_expert, b_router_expert,
           w_expert_gate, w_expert_up, w_expert_down, g_ple, w_ple_gate, w_ple_proj,
           g_final, _stage=99, _dbg=False):
    f = lambda a: np.ascontiguousarray(np.asarray(a, dtype=np.float32))
    x = f(x)
    p = f(p)
    shared = {
        "g_mix": f(g_mix[0]), "w_in": f(w_in[0]), "sink": f(sink[0]),
        "g_grp_a": f(g_grp_a[0]), "g_grp_b": f(g_grp_b[0]), "w_out": f(w_out[0]),
        "g_ffn": f(g_ffn[0]), "w_rg": f(w_router_group[0]), "b_rg": f(b_router_group[0]),
        "w_re": f(w_router_expert[0]), "b_re": f(b_router_expert[0]),
        "w_eg": f(w_expert_gate[0]), "w_eu": f(w_expert_up[0]), "w_ed": f(w_expert_down[0]),
        "g_ple": f(g_ple[0]), "w_pg": f(w_ple_gate[0]), "w_pp": f(w_ple_proj[0]),
        "g_final": f(g_final),
    }
    if _stage < 3:
        for k_ in ("w_eg", "w_eu", "w_ed"):
            shared.pop(k_)
    in_maps = []
    for c in range(8):
        b, hf = c // 2, c % 2
        if hf == 0:
            xl = x[b, 0:NLOC]
            plc = p[0, b, 0:NTOK]
        else:
            xl = x[b, 4096 - NLOC:4096][::-1]
            plc = p[0, b, NTOK:4096][::-1]
        m = dict(shared)
        m["xl"] = np.ascontiguousarray(xl)
        m["pl"] = np.ascontiguousarray(plc)
        in_maps.append(m)
    nc = _get_nc(_stage, _dbg)
    res = run_bass_kernel_spmd(nc, in_maps, core_ids=list(range(8)))
    out = np.empty((4, 4096, 1024), np.float32)
    dbg = np.empty((4, 4096, 1024), np.float32) if _dbg else None
    for c in range(8):
        b, hf = c // 2, c % 2
        yc = res.results[c]["y"]
        if hf == 0:
            out[b, 0:NTOK] = yc
        else:
            out[b, NTOK:4096] = yc[::-1]
        if _dbg:
            dc = res.results[c]["dbg"]
            if hf == 0:
                dbg[b, 0:NTOK] = dc
            else:
                dbg[b, NTOK:4096] = dc[::-1]
    if _dbg:
        return out, dbg
    return out
```

```python
import os
from contextlib import ExitStack
import numpy as np
import concourse.bass as bass
import concourse.mybir as mybir
from concourse.bass_utils import run_bass_kernel_spmd

F32 = mybir.dt.float32
BF16 = mybir.dt.bfloat16
I32 = mybir.dt.int32
U8 = mybir.dt.uint8
AF = mybir.ActivationFunctionType
ALU = mybir.AluOpType
AX = mybir.AxisListType

NTOK = 2048
NLOC = 3072
EPS = 1e-6
ARENA = 209920


class Buf:
    __slots__ = ("name", "w", "r")

    def __init__(self, name):
        self.name = name
        self.w = None
        self.r = {}


class Sched:
    ENG = ("pe", "act", "dve", "pool", "sp")

    def __init__(self, nc, stack):
        self.nc = nc
        self.stack = stack
        self.sems = {}
        self.cnt = {}
        self.seen = {e: {} for e in self.ENG}
        self.prog = {e: [] for e in self.ENG}
        for e in ("pe", "act", "dve", "pool"):
            self.sems[e] = stack.enter_context(nc.semaphore("s_" + e))
            self.cnt[e] = 0

    def _need(self, eng, toks):
        need = {}
        for t in toks:
            if t is None:
                continue
            k, v = t
            if k == eng and eng == "pe":
                continue
            if self.seen[eng].get(k, 0) >= v:
                continue
            if need.get(k, 0) < v:
                need[k] = v
        for k, v in need.items():
            self.seen[eng][k] = v
        return list(need.items())

    @staticmethod
    def _deps(reads, writes):
        toks = []
        for b in reads:
            toks.append(b.w)
        for b in writes:
            toks.append(b.w)
            for k, v in b.r.items():
                toks.append((k, v))
        return toks

    def op(self, eng, fn, reads=(), writes=()):
        waits = self._need(eng, self._deps(reads, writes))
        self.cnt[eng] += 1
        tok = (eng, self.cnt[eng])
        self._emit_now(eng, waits, fn, (eng, 1))
        for b in reads:
            if b.r.get(eng, 0) < tok[1]:
                b.r[eng] = tok[1]
        for b in writes:
            b.w = tok
            b.r = {}
        return tok

    def dma(self, q, fn, reads=(), writes=(), sem=None):
        if sem is None:
            sem = "d_" + (writes[0].name if writes else reads[0].name)
        if sem not in self.sems:
            self.sems[sem] = self.stack.enter_context(self.nc.semaphore(sem))
            self.cnt[sem] = 0
        waits = self._need(q, self._deps(reads, writes))
        self.cnt[sem] += 16
        tok = (sem, self.cnt[sem])
        self._emit_now(q, waits, fn, (sem, 16))
        for b in reads:
            if b.r.get(sem, 0) < tok[1]:
                b.r[sem] = tok[1]
        for b in writes:
            b.w = tok
            b.r = {}
        return tok

    def _emit_now(self, eng, waits, fn, inc):
        nc = self.nc
        eo = {"pe": nc.tensor, "act": nc.scalar, "dve": nc.vector, "pool": nc.gpsimd, "sp": nc.sync}[eng]
        for k, v in waits:
            eo.wait_ge(self.sems[k], v)
        if fn is not None:
            fn(eo).then_inc(self.sems[inc[0]], inc[1])

    def barrier(self):
        toks = [(k, v) for k, v in self.cnt.items() if v > 0]
        for e in self.ENG:
            waits = self._need(e, toks)
            if waits:
                self._emit_now(e, waits, None, None)

    def emit(self):
        return


class Arena:
    def __init__(self, nc, nbytes):
        self.t = nc.alloc_sbuf_tensor("arena", [128, nbytes], U8)
        self.n = nbytes
        self.top = 0

    def alloc(self, free, dtype):
        es = 4 if dtype in (F32, I32) else 2
        n = es
        for s in free:
            n *= s
        off = (self.top + 63) // 64 * 64
        assert off + n <= self.n, ("arena overflow", off + n, self.n)
        self.top = off + n
        v = self.t[:, off:off + n].bitcast(dtype)
        if len(free) == 2:
            v = v.rearrange("p (a b) -> p a b", a=free[0])
        elif len(free) == 3:
            v = v.rearrange("p (a b c) -> p a b c", a=free[0], b=free[1])
        return v


def build_nc(stage=99, dbg=False):
    nc = bass.Bass("TRN2", target_bir_lowering=False)

    def din(name, shape):
        return nc.dram_tensor(name, list(shape), F32, kind="ExternalInput").ap()

    xl = din("xl", [NLOC, 1024])
    pl = din("pl", [NTOK, 256])
    g_mix = din("g_mix", [1024])
    w_in = din("w_in", [1024, 2304])
    sink = din("sink", [8])
    g_grp_a = din("g_grp_a", [512])
    g_grp_b = din("g_grp_b", [512])
    w_out = din("w_out", [1024, 1024])
    g_ffn = din("g_ffn", [1024])
    w_rg = din("w_rg", [1024, 4])
    b_rg = din("b_rg", [4])
    w_re = din("w_re", [1024, 32])
    b_re = din("b_re", [32])
    if stage >= 3:
        w_eg = din("w_eg", [32, 1024, 512])
        w_eu = din("w_eu", [32, 1024, 512])
        w_ed = din("w_ed", [32, 512, 1024])
    g_ple = din("g_ple", [1024])
    w_pg = din("w_pg", [1024, 1024])
    w_pp = din("w_pp", [256, 1024])
    g_final = din("g_final", [1024])
    y = nc.dram_tensor("y", [NTOK, 1024], F32, kind="ExternalOutput").ap()
    dbg_o = None
    if dbg:
        dbg_o = nc.dram_tensor("dbg", [NTOK, 1024], F32, kind="ExternalOutput").ap()

    with ExitStack() as stack:
        S = Sched(nc, stack)
        A = Arena(nc, ARENA)
        ps = nc.alloc_psum_tensor("ps", [128, 8, 512], F32)
        b_ps = [Buf("ps%d" % i) for i in range(8)]

        def psb(i):
            return ps[:, i, :]

        def psb16(i):
            return ps[:, i, :].bitcast(BF16)

        ident_f = A.alloc((128,), F32)
        ident_b = A.alloc((128,), BF16)
        gall = A.alloc((128,), F32)
        gT = A.alloc((32,), F32)
        wr = A.alloc((8, 36), F32)
        rbias = A.alloc((36,), F32)
        esink = A.alloc((8,), F32)
        junk4 = A.alloc((1024,), BF16)
        st4 = A.alloc((6, 16), F32)
        stm = A.alloc((3, 16), F32)
        b_junk4, b_st4, b_stm = Buf("junk4"), Buf("st4"), Buf("stm")
        junk3 = A.alloc((512,), BF16)
        st3 = A.alloc((3, 32), F32)
        b_junk3, b_st3 = Buf("junk3"), Buf("st3")
        dw = A.alloc((16, 32), F32)
        b_ident, b_gall, b_gT, b_wr, b_rbias, b_esink, b_gfin = [Buf(n) for n in
            ("ident", "gall", "gT", "wr", "rbias", "esink", "gfin")]
        b_dw = [Buf("dw%d" % t) for t in range(16)]

        S.op("pool", lambda e: e.memset(ident_f, 0.0), writes=[b_ident])
        S.op("pool", lambda e: e.affine_select(out=ident_f, in_=ident_f, pattern=[[-1, 128]],
                                               compare_op=ALU.not_equal, fill=1.0, base=0, channel_multiplier=1),
             reads=[b_ident], writes=[b_ident])
        S.op("dve", lambda e: e.tensor_copy(out=ident_b, in_=ident_f), reads=[b_ident], writes=[b_ident])
        S.op("pool", lambda e: e.memset(gall, 0.0), writes=[b_gall])
        for i, (g, nk) in enumerate(((g_mix, 8), (g_ffn, 8), (g_ple, 8))):
            S.dma("sp", lambda e, g=g, i=i: e.dma_start(out=gall[8 * i:8 * i + 8, :],
                                                        in_=g.rearrange("(k p) -> k p", p=128)),
                  writes=[b_gall])
        S.dma("sp", lambda e: e.dma_start(out=gall[24:28, :], in_=g_grp_a.rearrange("(k p) -> k p", p=128)),
              writes=[b_gall])
        S.dma("sp", lambda e: e.dma_start(out=gall[28:32, :], in_=g_grp_b.rearrange("(k p) -> k p", p=128)),
              writes=[b_gall])
        S.dma("sp", lambda e: e.dma_start(out=wr[:, :, 0:4], in_=w_rg.rearrange("(k p) g -> p k g", p=128)),
              writes=[b_wr])
        S.dma("sp", lambda e: e.dma_start(out=wr[:, :, 4:36], in_=w_re.rearrange("(k p) g -> p k g", p=128)),
              writes=[b_wr])
        S.dma("sp", lambda e: e.dma_start(out=rbias[:, 0:4], in_=b_rg.partition_broadcast(128)),
              writes=[b_rbias])
        S.dma("sp", lambda e: e.dma_start(out=rbias[:, 4:36], in_=b_re.partition_broadcast(128)),
              writes=[b_rbias])
        S.dma("sp", lambda e: e.dma_start(out=esink, in_=sink.partition_broadcast(128)),
              writes=[b_esink])
        S.op("act", lambda e: e.activation(out=esink, in_=esink, func=AF.Exp), reads=[b_esink], writes=[b_esink])
        S.op("pe", lambda e: e.transpose(out=psb(0)[:, 0:32], in_=gall[0:32, :], identity=ident_f[0:32, 0:32]),
             reads=[b_gall, b_ident], writes=[b_ps[0]])
        S.op("dve", lambda e: e.tensor_copy(out=gT, in_=psb(0)[:, 0:32]), reads=[b_ps[0]], writes=[b_gT])

        base = A.top

        def rstd_ops(ssq, tmp, out, n, bufs):
            S.op("dve", lambda e: e.tensor_scalar(out=tmp, in0=ssq, scalar1=1.0 / n, scalar2=EPS,
                                                  op0=ALU.mult, op1=ALU.add), reads=bufs, writes=bufs)
            S.op("act", lambda e: e.activation(out=tmp, in_=tmp, func=AF.Sqrt), reads=bufs, writes=bufs)
            S.op("dve", lambda e: e.reciprocal(out=out, in_=tmp), reads=bufs, writes=bufs)

        R1 = A.alloc((65536 // 2,), BF16)
        QT_A = R1[:, 0:8192].rearrange("p (a b) -> p a b", a=4)
        KT_A = R1[:, 8192:8192 + 12288].rearrange("p (a b) -> p a b", a=4)
        VT_A = R1[:, 20480:20480 + 12288].rearrange("p (a b) -> p a b", a=4)
        x1 = R1.bitcast(F32).rearrange("p (t d) -> p t d", t=16)
        r2_off = A.top
        QT_B = A.alloc((4, 2048), BF16)
        KT_B = A.alloc((2, 2176), BF16)
        VT_B = A.alloc((2176,), BF16)
        r3_off = A.top
        b_QA, b_KA, b_VA, b_QB, b_KB, b_VB = [Buf(n) for n in ("QA", "KA", "VA", "QB", "KB", "VB")]

        hT = A.alloc((8, NLOC), BF16)
        xs = [A.alloc((1024,), F32) for _ in range(2)]
        xn = [A.alloc((1024,), BF16) for _ in range(2)]
        junk = A.alloc((1024,), BF16)
        wblk = [A.alloc((8, 768), BF16) for _ in range(2)]
        st1 = A.alloc((24, 4), F32)
        wkb = A.alloc((8, 2, 128), BF16)
        b_wkb = Buf("wkb")
        for g_ in range(2):
            for hh_ in range(2):
                S.dma("pool", lambda e, g_=g_, hh_=hh_: e.dma_start(
                    out=wkb[:, :, g_, 64 * hh_:64 * hh_ + 64],
                    in_=w_in[:, 2048 + 64 * g_:2048 + 64 * g_ + 64].rearrange("(k p) c -> p k c", p=128)),
                    writes=[b_wkb])
        b_hT = [Buf("hT%d" % i) for i in range(6)]
        b_xs = [Buf("xs%d" % i) for i in range(2)]
        b_xn = [Buf("xn%d" % i) for i in range(2)]
        b_junk = Buf("junk")
        b_wblk = [Buf("wblk%d" % i) for i in range(2)]
        b_st1 = [Buf("st1_%d" % i) for i in range(24)]

        def load_wblk(bi):
            S.dma("pool", lambda e, bi=bi: e.dma_start(
                out=wblk[bi % 2], in_=w_in[:, 768 * bi:768 * (bi + 1)].rearrange("(k p) c -> p k c", p=128)),
                writes=[b_wblk[bi % 2]])

        load_wblk(0)
        load_wblk(1)

        gmb = gT[:, 0:8].unsqueeze(2).to_broadcast([128, 8, 128])
        for t in range(24):
            s = t % 2
            S.dma("sp", lambda e, t=t, s=s: e.dma_start(out=xs[s], in_=xl[128 * t:128 * (t + 1), :]),
                  writes=[b_xs[s]])
            S.op("act", lambda e, t=t, s=s: e.activation(out=junk, in_=xs[s], func=AF.Square,
                                                         accum_out=st1[:, t, 0:1]),
                 reads=[b_xs[s]], writes=[b_junk, b_st1[t]])
            rstd_ops(st1[:, t, 0:1], st1[:, t, 1:2], st1[:, t, 2:3], 1024, [b_st1[t]])
            S.op("dve", lambda e, t=t, s=s: e.tensor_scalar(out=xn[s], in0=xs[s], scalar1=st1[:, t, 2:3],
                                                            scalar2=None, op0=ALU.mult),
                 reads=[b_xs[s], b_st1[t]], writes=[b_xn[s]])
            pb = t % 2
            for k in range(8):
                S.op("pe", lambda e, k=k, s=s, pb=pb: e.transpose(out=psb16(pb)[:, 128 * k:128 * (k + 1)],
                                                                  in_=xn[s][:, 128 * k:128 * (k + 1)],
                                                                  identity=ident_b),
                     reads=[b_xn[s], b_ident], writes=[b_ps[pb]])
            S.op("dve", lambda e, t=t, pb=pb: e.tensor_tensor(
                out=hT[:, :, 128 * t:128 * (t + 1)],
                in0=psb16(pb).rearrange("p (k t) -> p k t", k=8), in1=gmb, op=ALU.mult),
                reads=[b_ps[pb], b_gT], writes=[b_hT[t // 4]])

        evac_rr = [0]

        def evac(out_ap, in_ap, reads, writes):
            evac_rr[0] += 1
            if evac_rr[0] % 2 == 0:
                S.op("act", lambda e: e.copy(out=out_ap, in_=in_ap), reads=reads, writes=writes)
            else:
                S.op("dve", lambda e: e.tensor_copy(out=out_ap, in_=in_ap), reads=reads, writes=writes)

        pbank = [2]

        def proj_fm(wb, lhs_fn, dst, dbuf, tok_blocks):
            for (t0, n) in tok_blocks:
                pb = pbank[0]
                pbank[0] = 2 + (pbank[0] - 2 + 1) % 6
                for k in range(8):
                    S.op("pe", lambda e, k=k, pb=pb, t0=t0, n=n: e.matmul(
                        psb(pb)[:, 0:n], lhsT=lhs_fn(k), rhs=hT[:, k, t0:t0 + n], start=(k == 0), stop=(k == 7)),
                        reads=[wb, b_hT[t0 // 512]], writes=[b_ps[pb]])
                evac(dst[:, t0:t0 + n], psb(pb)[:, 0:n], [b_ps[pb]], [dbuf])

        tb_own = [(512 * i, 512) for i in range(4)]
        tb_all = [(512 * i, 512) for i in range(6)]
        tb_b = [(512 * i, 512) for i in range(4)] + [(2048, 128)]
        w0, w1 = wblk[0], wblk[1]
        for j in range(4):
            proj_fm(b_wblk[0], lambda k, j=j: w0[:, k, 128 * j:128 * (j + 1)], QT_A[:, j, :], b_QA, tb_own)
        for j in range(2):
            proj_fm(b_wblk[0], lambda k, j=j: w0[:, k, 512 + 128 * j:512 + 128 * (j + 1)], KT_A[:, j, :], b_KA, tb_all)
        load_wblk(2)
        for j in range(2):
            proj_fm(b_wblk[1], lambda k, j=j: w1[:, k, 128 * j:128 * (j + 1)], KT_A[:, 2 + j, :], b_KA, tb_all)
        for j in range(4):
            proj_fm(b_wblk[1], lambda k, j=j: w1[:, k, 256 + 128 * j:256 + 128 * (j + 1)], VT_A[:, j, :], b_VA, tb_all)
        for j in range(4):
            proj_fm(b_wblk[0], lambda k, j=j: w0[:, k, 128 * j:128 * (j + 1)], QT_B[:, j, :], b_QB, tb_own)
        for g in range(2):
            proj_fm(b_wkb, lambda k, g=g: wkb[:, k, g, :], KT_B[:, g, :], b_KB, tb_b)
        proj_fm(b_wblk[0], lambda k: w0[:, k, 640:768], VT_B, b_VB, tb_b)

        S.barrier()

        A.top = r3_off
        o_tm = A.alloc((16, 1024), BF16)
        after_o = A.top
        WA = A.alloc((24, 256), BF16)
        WB = A.alloc((8, 384), BF16)
        Vc = [A.alloc((32, 2, 65), BF16) for _ in range(2)]
        acc = [A.alloc((2048,), F32) for _ in range(2)]
        NEB = 4
        Eb = [A.alloc((512,), F32) for _ in range(NEB)]
        PT = [A.alloc((512,), BF16) for _ in range(NEB)]
        trel_i = A.alloc((128,), I32)
        trel = A.alloc((128,), F32)
        aabs = A.alloc((128,), F32)
        amask = A.alloc((128,), F32)
        etmp = [A.alloc((128,), F32) for _ in range(2)]
        rden = A.alloc((2, 16), F32)
        b_o = [Buf("o%d" % t) for t in range(16)]
        b_WA, b_WB = Buf("WA"), Buf("WB")
        b_Vc = [Buf("Vc0"), Buf("Vc1")]
        b_acc = [Buf("acc0"), Buf("acc1")]
        b_E = [Buf("E%d" % i) for i in range(NEB)]
        b_PT = [Buf("PT%d" % i) for i in range(NEB)]
        b_trel, b_aabs, b_amask = Buf("trel"), Buf("aabs"), Buf("amask")
        b_etmp = [Buf("etmp0"), Buf("etmp1")]
        b_rden = [Buf("rden0"), Buf("rden1")]

        S.op("pool", lambda e: e.iota(trel_i, pattern=[[1, 128]], base=0, channel_multiplier=-1), writes=[b_trel])
        S.op("dve", lambda e: e.tensor_copy(out=trel, in_=trel_i), reads=[b_trel], writes=[b_trel])
        S.op("pool", lambda e: e.memset(Vc[0][:, :, :, 64:65], 1.0), writes=[b_Vc[0]])
        S.op("pool", lambda e: e.memset(Vc[1][:, :, :, 64:65], 1.0), writes=[b_Vc[1]])
        wcount = [0]

        aab, amk, b_ab = {}, {}, {}
        for off, half in ((64, 64), (-64, 64), (128, 128), (0, 128), (-128, 128)):
            aab[off] = A.alloc((128,), F32)
            amk[off] = A.alloc((128,), F32)
            b_ab[off] = Buf("ab%d" % off)
            S.op("dve", lambda e, off=off: e.tensor_scalar(out=etmp[0], in0=trel, scalar1=float(off), scalar2=None,
                                                           op0=ALU.add),
                 reads=[b_trel], writes=[b_etmp[0]])
            S.op("dve", lambda e: e.tensor_scalar(out=etmp[1], in0=etmp[0], scalar1=-1.0, scalar2=None,
                                                  op0=ALU.mult),
                 reads=[b_etmp[0]], writes=[b_etmp[1]])
            S.op("dve", lambda e, off=off: e.tensor_max(out=aab[off], in0=etmp[0], in1=etmp[1]),
                 reads=[b_etmp[0], b_etmp[1]], writes=[b_ab[off]])
            S.op("dve", lambda e, off=off, half=half: e.tensor_single_scalar(
                out=amk[off], in_=aab[off], scalar=float(half) + 0.5, op=ALU.is_le),
                reads=[b_ab[off]], writes=[b_ab[off]])

        def gen_entries(off, entries):
            for (ns, out_ap, wbuf) in entries:
                i = wcount[0] % 2
                wcount[0] += 1
                S.op("act", lambda e, ns=ns, i=i: e.activation(out=etmp[i], in_=aab[off], func=AF.Exp,
                                                               scale=float(ns)),
                     reads=[b_ab[off]], writes=[b_etmp[i]])
                S.op("dve", lambda e, i=i, out_ap=out_ap: e.tensor_tensor(out=out_ap, in0=etmp[i], in1=amk[off],
                                                                          op=ALU.mult),
                     reads=[b_etmp[i], b_ab[off]], writes=[wbuf])

        CFG = ((128, 1), (512, 4), (2048, 16))
        slopes = [2.0 ** (-(h + 1)) for h in range(8)]
        b_WAp = [Buf("WAp%d" % i) for i in range(4)]

        def gen_pair_tables(jp):
            for part, off in ((0, 64), (1, -64)):
                ent = []
                for c, (_, d) in enumerate(CFG):
                    for h in (2 * jp, 2 * jp + 1):
                        ent.append((-slopes[h] * d, WA[:, c * 8 + h, 128 * part:128 * (part + 1)], b_WAp[jp]))
                gen_entries(off, ent)

        def gen_b_tables():
            for part, off in ((0, 128), (1, 0), (2, -128)):
                gen_entries(off, [(-slopes[h], WB[:, h, 128 * part:128 * (part + 1)], b_WB) for h in range(8)])

        gen_pair_tables(0)

        rot = {"st": 0, "e": 0, "pt": 0, "ot": 0}
        pending = []
        ST_BANKS = (0, 1, 2, 3)
        OT_BANKS = (4, 5)
        VTR_BANK = 6
        EP_BANKS = (6, 7, 6)
        SKEW = 2

        def submit(front, back):
            front()
            if len(pending) >= SKEW:
                pending.pop(0)()
            pending.append(back)

        def flush():
            while pending:
                for bk_ in pending.pop(0):
                    bk_()

        jobq = {0: [], 1: []}
        cur_q = [None]

        def run_job(blocks, actmul, segs, evac_fn, Kb, Qb, Vb, accb):
            st_ = {}

            def assign():
                st_["sb"] = ST_BANKS[rot["st"] % len(ST_BANKS)]
                rot["st"] += 1
                st_["ei"] = rot["e"] % NEB
                rot["e"] += 1
                st_["pi"] = rot["pt"] % NEB
                rot["pt"] += 1

            def qk():
                sb = st_["sb"]
                return [(lambda c0=c0, w=w, nk=nk, kt_ap=kt_ap, q_ap=q_ap: S.op(
                    "pe", lambda e: e.matmul(psb(sb)[0:nk, c0:c0 + w], lhsT=kt_ap, rhs=q_ap, start=True, stop=True),
                    reads=[Kb, Qb], writes=[b_ps[sb]])) for (c0, w, nk, kt_ap, q_ap) in blocks]

            def rest():
                sb, ei, pi = st_["sb"], st_["ei"], st_["pi"]
                for (rows, vf, w_ap, wbuf) in actmul:
                    S.op("act", lambda e, rows=rows, vf=vf: e.activation(
                        out=vf(Eb[ei])[0:rows], in_=vf(psb(sb))[0:rows], func=AF.Exp, scale=0.125),
                        reads=[b_ps[sb]], writes=[b_E[ei]])
                for (rows, vf, w_ap, wbuf) in actmul:
                    S.op("dve", lambda e, rows=rows, vf=vf, w_ap=w_ap: e.tensor_tensor(
                        out=vf(PT[pi])[0:rows], in0=vf(Eb[ei])[0:rows], in1=w_ap[0:rows], op=ALU.mult),
                        reads=[b_E[ei], wbuf], writes=[b_PT[pi]])

            def back():
                pi = st_["pi"]
                ob = OT_BANKS[rot["ot"] % 2]
                rot["ot"] += 1
                for (ocol, w, parts) in segs:
                    for n_i, (nk, lhsT_ap, pc0) in enumerate(parts):
                        S.op("pe", lambda e, ocol=ocol, w=w, nk=nk, lhsT_ap=lhsT_ap, pc0=pc0, n_i=n_i,
                             last=(n_i == len(parts) - 1): e.matmul(
                            psb(ob)[0:65, ocol:ocol + w], lhsT=lhsT_ap, rhs=PT[pi][0:nk, pc0:pc0 + w],
                            start=(n_i == 0), stop=last),
                            reads=[b_PT[pi], Vb], writes=[b_ps[ob]])
                out_ap, in_ap, is_add = evac_fn(psb(ob))
                if is_add:
                    S.op("dve", lambda e: e.tensor_tensor(out=out_ap, in0=out_ap, in1=in_ap, op=ALU.add),
                         reads=[b_ps[ob], accb], writes=[accb])
                else:
                    S.op("dve", lambda e: e.tensor_copy(out=out_ap, in_=in_ap), reads=[b_ps[ob]], writes=[accb])

            job = (assign, qk, rest, back)
            if cur_q[0] is None:
                emit_group([job])
            else:
                jobq[cur_q[0]].append(job)

        def emit_group(jobs):
            for j in jobs:
                j[0]()
            qks = [j[1]() for j in jobs]
            for i in range(max(len(q) for q in qks)):
                for q in qks:
                    if i < len(q):
                        q[i]()
            for j in jobs:
                j[2]()
            if len(pending) >= 1:
                for bk_ in pending.pop(0):
                    bk_()
            pending.append([j[3] for j in jobs])

        def emit_pairs():
            n0, n1 = len(jobq[0]), len(jobq[1])
            for i in range(max(n0, n1)):
                grp = []
                if i < n0:
                    grp.append(jobq[0][i])
                if i < n1:
                    grp.append(jobq[1][i])
                emit_group(grp)
            jobq[0], jobq[1] = [], []
            cur_q[0] = None

        def flat(v):
            return v

        def epilogue(acc_i, ch0, sink_h):
            a = acc[acc_i]
            groups = ((0, 7), (7, 7), (14, 2))
            for gi, (t0, n) in enumerate(groups):
                bk = EP_BANKS[gi]
                pv = psb(bk)[:, 0:455].rearrange("p (t c) -> p t c", c=65)
                for tt in range(n):
                    t = t0 + tt
                    S.op("pe", lambda e, pv=pv, tt=tt, t=t: e.transpose(
                        out=pv[:, tt, :], in_=a[0:65, 128 * t:128 * (t + 1)], identity=ident_f[0:65, 0:65]),
                        reads=[b_acc[acc_i], b_ident], writes=[b_ps[bk]])
                rd = rden[:, acc_i, t0:t0 + n]
                if sink_h is not None:
                    S.op("dve", lambda e, pv=pv, n=n, rd=rd: e.tensor_scalar(
                        out=rd, in0=pv[:, 0:n, 64], scalar1=esink[:, sink_h:sink_h + 1], scalar2=None, op0=ALU.add),
                        reads=[b_ps[bk], b_esink], writes=[b_rden[acc_i]])
                    S.op("dve", lambda e, rd=rd: e.reciprocal(out=rd, in_=rd), reads=[b_rden[acc_i]],
                         writes=[b_rden[acc_i]])
                else:
                    S.op("dve", lambda e, pv=pv, n=n, rd=rd: e.reciprocal(out=rd, in_=pv[:, 0:n, 64]),
                         reads=[b_ps[bk]], writes=[b_rden[acc_i]])
                S.op("dve", lambda e, pv=pv, n=n, rd=rd, t0=t0: e.tensor_tensor(
                    out=o_tm[:, t0:t0 + n, ch0:ch0 + 64], in0=pv[:, 0:n, 0:64],
                    in1=rd.unsqueeze(2).to_broadcast([128, n, 64]), op=ALU.mult),
                    reads=[b_ps[bk], b_rden[acc_i]], writes=[b_o[t] for t in range(t0, t0 + n)])

        vrot = [0]

        def build_v(src_fn, tiles, vbuf_i, nheads_cols):
            vb = Vc[vbuf_i]
            i = 0
            while i < len(tiles):
                grp = tiles[i:i + 8]
                for s_, (tidx, nk, src) in enumerate(grp):
                    S.op("pe", lambda e, s_=s_, nk=nk, src=src: e.transpose(
                        out=psb16(VTR_BANK)[0:nk, 128 * s_:128 * (s_ + 1)], in_=src, identity=ident_b),
                        reads=[src_fn, b_ident], writes=[b_ps[VTR_BANK]])
                t0 = grp[0][0]
                n = len(grp)
                S.op("act", lambda e, t0=t0, n=n: e.copy(
                    out=vb[:, t0:t0 + n, :, 0:64],
                    in_=psb16(VTR_BANK)[:, 0:128 * n].rearrange("p (t h c) -> p t h c", t=n, h=2)),
                    reads=[b_ps[VTR_BANK]], writes=[b_Vc[vbuf_i]])
                i += 8

        def dil(ap2, d):
            return ap2.rearrange("p (u d) -> p d u", d=d)

        def e3(n, w):
            return lambda tl: tl[:, 0:n * w].rearrange("p (n w) -> p n w", n=n)

        vsel = [0]
        if stage >= 2:
            for jj in range(4):
                if jj + 1 < 4:
                    gen_pair_tables(jj + 1)
                if jj == 2:
                    gen_b_tables()
                b_WA = b_WAp[jj]
                for c, (_, d) in enumerate(CFG):
                    N = NTOK // d // 128
                    nt = N + 1
                    vi = vsel[0] % 2
                    vsel[0] += 1
                    vsrc = dil(VT_A[:, jj, :], d)
                    tiles = []
                    for r in range(d):
                        for j in range(nt):
                            nk = 64 if (d == 16 and j == 1) else 128
                            tiles.append((r * nt + j, nk, vsrc[:, r, 128 * j:128 * j + nk]))
                    build_v(b_VA, tiles, vi, 2)
                    vb = Vc[vi]
                    for e_ in range(2):
                        cur_q[0] = e_
                        h = 2 * jj + e_
                        pr = slice(64 * e_, 64 * e_ + 64)
                        qd = dil(QT_A[pr, jj, :], d)
                        kd = dil(KT_A[pr, jj, :], d)
                        accv = dil(acc[e_][0:65, :], d)
                        hc = c * 8 + h
                        Wfull = WA[:, hc, :]
                        is_add = (c > 0)
                        nkL = 64 if d == 16 else 128

                        def ktile(r, j, nk=128):
                            return kd[:, r, 128 * j:128 * j + nk]

                        def vt(r, j, nk=128):
                            return vb[0:nk, r * nt + j, e_, :]

                        for r0 in range(0, d, 8):
                            n = min(8, d - r0)
                            blocks, segs = [], []
                            for rr in range(n):
                                r = r0 + rr
                                blocks.append((64 * rr, 64, 128, ktile(r, 0), qd[:, r, 0:64]))
                                segs.append((64 * rr, 64, [(128, vt(r, 0), 64 * rr)]))
                            am = [(128, e3(n, 64), Wfull[:, 192:256].unsqueeze(1).to_broadcast([128, n, 64]), b_WA)]
                            run_job(blocks, am, segs,
                                    lambda ot, r0=r0, n=n, accv=accv, is_add=is_add: (
                                        accv[:, r0:r0 + n, 0:64], e3(n, 64)(ot)[0:65], is_add),
                                    b_KA, b_QA, b_Vc[vi], b_acc[e_])
                        full = list(range(1, N))
                        for r in range(d):
                            ii = 0
                            while ii + 1 < len(full):
                                i0 = full[ii]
                                blocks, segs = [], []
                                for sgi in range(2):
                                    i_ = i0 + sgi
                                    q_ap = qd[:, r, 128 * i_ - 64:128 * i_ + 64]
                                    blocks.append((256 * sgi, 128, 128, ktile(r, i_ - 1), q_ap))
                                    blocks.append((256 * sgi + 128, 128, 128, ktile(r, i_), q_ap))
                                    segs.append((128 * sgi, 128, [(128, vt(r, i_ - 1), 256 * sgi),
                                                                  (128, vt(r, i_), 256 * sgi + 128)]))
                                am = [(128, e3(2, 256), Wfull.unsqueeze(1).to_broadcast([128, 2, 256]), b_WA)]
                                run_job(blocks, am, segs,
                                        lambda ot, r=r, i0=i0, accv=accv, is_add=is_add: (
                                            accv[:, r:r + 1, 128 * i0 - 64:128 * i0 + 192], e3(1, 256)(ot)[0:65],
                                            is_add),
                                        b_KA, b_QA, b_Vc[vi], b_acc[e_])
                                ii += 2
                        if len(full) % 2 == 1:
                            i_ = full[-1]
                            for r0 in range(0, d, 2):
                                n = min(2, d - r0)
                                blocks, segs = [], []
                                for rr in range(n):
                                    r = r0 + rr
                                    q_ap = qd[:, r, 128 * i_ - 64:128 * i_ + 64]
                                    blocks.append((256 * rr, 128, 128, ktile(r, i_ - 1), q_ap))
                                    blocks.append((256 * rr + 128, 128, 128, ktile(r, i_), q_ap))
                                    segs.append((128 * rr, 128, [(128, vt(r, i_ - 1), 256 * rr),
                                                                 (128, vt(r, i_), 256 * rr + 128)]))
                                am = [(128, e3(n, 256), Wfull.unsqueeze(1).to_broadcast([128, n, 256]), b_WA)]
                                run_job(blocks, am, segs,
                                        lambda ot, r0=r0, n=n, i_=i_, accv=accv, is_add=is_add: (
                                            accv[:, r0:r0 + n, 128 * i_ - 64:128 * i_ + 64], e3(n, 128)(ot)[0:65],
                                            is_add),
                                        b_KA, b_QA, b_Vc[vi], b_acc[e_])
                        for r0 in range(0, d, 4):
                            n = min(4, d - r0)
                            blocks, segs = [], []
                            for rr in range(n):
                                r = r0 + rr
                                q_ap = qd[:, r, 128 * N - 64:128 * N]
                                blocks.append((128 * rr, 64, 128, ktile(r, N - 1), q_ap))
                                blocks.append((128 * rr + 64, 64, nkL, ktile(r, N, nkL), q_ap))
                                segs.append((64 * rr, 64, [(128, vt(r, N - 1), 128 * rr),
                                                            (nkL, vt(r, N, nkL), 128 * rr + 64)]))

                            def vprev(tl, n=n):
                                return tl[:, 0:128 * n].rearrange("p (n a w) -> p n a w", n=n, a=2)[:, :, 0, :]

                            def vcur(tl, n=n):
                                return tl[:, 0:128 * n].rearrange("p (n a w) -> p n a w", n=n, a=2)[:, :, 1, :]

                            am = [(128, vprev, Wfull[:, 0:64].unsqueeze(1).to_broadcast([128, n, 64]), b_WA),
                                  (nkL, vcur, Wfull[:, 128:192].unsqueeze(1).to_broadcast([128, n, 64]), b_WA)]
                            run_job(blocks, am, segs,
                                    lambda ot, r0=r0, n=n, N=N, accv=accv, is_add=is_add: (
                                        accv[:, r0:r0 + n, 128 * N - 64:128 * N], e3(n, 64)(ot)[0:65], is_add),
                                    b_KA, b_QA, b_Vc[vi], b_acc[e_])
                    emit_pairs()
                flush()
                for e_ in range(2):
                    epilogue(e_, 64 * (2 * jj + e_), None)
                if jj == 3:
                    for t in range(16):
                        S.op("act", lambda e, t=t: e.activation(
                            out=junk3, in_=o_tm[:, t, 0:512], func=AF.Square, accum_out=st3[:, 0, t:t + 1]),
                            reads=[b_o[t]], writes=[b_junk3, b_st3])

            vi = vsel[0] % 2
            vsel[0] += 1
            tiles = [(j, 128, VT_B[:, 128 * j:128 * (j + 1)]) for j in range(17)]
            build_v(b_VB, tiles, vi, 2)
            vb = Vc[vi]
            for h in range(8):
                jj, e_, g = h // 2, h % 2, h // 4
                cur_q[0] = e_
                pr = slice(64 * e_, 64 * e_ + 64)
                ai = h % 2
                for i_ in range(16):
                    jlo, jhi = max(0, i_ - 1), min(16, i_ + 1)
                    nb = jhi - jlo + 1
                    q_ap = QT_B[pr, jj, 128 * i_:128 * (i_ + 1)]
                    blocks, parts = [], []
                    for bi, j in enumerate(range(jlo, jhi + 1)):
                        blocks.append((128 * bi, 128, 128, KT_B[pr, g, 128 * j:128 * (j + 1)], q_ap))
                        parts.append((128, vb[:, j, g, :], 128 * bi))
                    wo = 128 * (jlo - i_ + 1)
                    am = [(128, (lambda tl, nb=nb: tl[:, 0:128 * nb]), WB[:, h, wo:wo + 128 * nb], b_WB)]
                    run_job(blocks, am, [(0, 128, parts)],
                            lambda ot, i_=i_, ai=ai: (acc[ai][0:65, 128 * i_:128 * (i_ + 1)], ot[0:65, 0:128], False),
                            b_KB, b_QB, b_Vc[vi], b_acc[ai])
                if e_ == 1:
                    emit_pairs()
                    flush()
                    epilogue(0, 512 + 64 * (h - 1), h - 1)
                    epilogue(1, 512 + 64 * h, h)

        S.barrier()

        def pipeline(n, stages):
            ns = len(stages)
            for i in range(n + ns - 1):
                for s_, f_ in enumerate(stages):
                    t_ = i - s_
                    if 0 <= t_ < n:
                        f_(t_)

        A.top = r2_off
        wout_b = A.alloc((8, 1024), BF16)
        xs3 = [A.alloc((1024,), F32) for _ in range(3)]
        assert A.top <= r3_off
        A.top = after_o
        mixn = [A.alloc((1024,), BF16) for _ in range(3)]
        mixT = [A.alloc((8, 128), BF16) for _ in range(2)]
        b_wout = Buf("wout")
        b_xs3 = [Buf("xs3_%d" % i) for i in range(3)]
        b_mixn = [Buf("mixn%d" % i) for i in range(3)]
        b_mixT = [Buf("mixT0"), Buf("mixT1")]
        b_x1 = [[Buf("x1_%d_%d" % (t, hf)) for hf in range(2)] for t in range(16)]

        S.dma("pool", lambda e: e.dma_start(out=wout_b, in_=w_out.rearrange("(k p) c -> p k c", p=128)),
              writes=[b_wout])
        ggb = gT[:, 24:32].unsqueeze(2).to_broadcast([128, 8, 128])
        if stage >= 2:
            for t in range(16):
                for gi in range(1, 2):
                    S.op("act", lambda e, t=t, gi=gi: e.activation(
                        out=junk3, in_=o_tm[:, t, 512 * gi:512 * (gi + 1)], func=AF.Square,
                        accum_out=st3[:, 0, 16 * gi + t:16 * gi + t + 1]),
                        reads=[b_o[t]], writes=[b_junk3, b_st3])
            rstd_ops(st3[:, 0, :], st3[:, 1, :], st3[:, 2, :], 512, [b_st3])

            def a3_s0(t):
                s = t % 3
                S.dma("sp", lambda e: e.dma_start(out=xs3[s], in_=xl[128 * t:128 * (t + 1), :]), writes=[b_xs3[s]])
                for gi in range(2):
                    S.op("dve", lambda e, gi=gi: e.tensor_scalar(
                        out=mixn[s][:, 512 * gi:512 * (gi + 1)], in0=o_tm[:, t, 512 * gi:512 * (gi + 1)],
                        scalar1=st3[:, 2, 16 * gi + t:16 * gi + t + 1], scalar2=None, op0=ALU.mult),
                        reads=[b_o[t], b_st3], writes=[b_mixn[s]])
                pb = t % 2
                for k in range(8):
                    S.op("pe", lambda e, k=k: e.transpose(
                        out=psb16(pb)[:, 128 * k:128 * (k + 1)], in_=mixn[s][:, 128 * k:128 * (k + 1)],
                        identity=ident_b), reads=[b_mixn[s], b_ident], writes=[b_ps[pb]])

            def a3_s1(t):
                pb = t % 2
                m = t % 2
                S.op("dve", lambda e: e.tensor_tensor(
                    out=mixT[m], in0=psb16(pb).rearrange("p (k t) -> p k t", k=8), in1=ggb, op=ALU.mult),
                    reads=[b_ps[pb], b_gT], writes=[b_mixT[m]])
                for hf in range(2):
                    ob = 2 + (2 * t + hf) % 4
                    for k in range(8):
                        S.op("pe", lambda e, k=k, hf=hf, ob=ob: e.matmul(
                            psb(ob), lhsT=mixT[m][:, k, :], rhs=wout_b[:, k, 512 * hf:512 * (hf + 1)],
                            start=(k == 0), stop=(k == 7)),
                            reads=[b_mixT[m], b_wout], writes=[b_ps[ob]])

            def a3_s2(t):
                s = t % 3
                for hf in range(2):
                    ob = 2 + (2 * t + hf) % 4
                    S.op("dve", lambda e, hf=hf, ob=ob: e.tensor_tensor(
                        out=x1[:, t, 512 * hf:512 * (hf + 1)], in0=xs3[s][:, 512 * hf:512 * (hf + 1)],
                        in1=psb(ob), op=ALU.add),
                        reads=[b_xs3[s], b_ps[ob]], writes=[b_x1[t][hf]])
                if stage >= 3:
                    S.op("act", lambda e: e.activation(out=junk4, in_=x1[:, t, :], func=AF.Square,
                                                       accum_out=stm[:, 0, t:t + 1]),
                         reads=b_x1[t], writes=[b_junk4, b_stm])

            pipeline(16, [a3_s0, a3_s1, a3_s2])
        else:
            for t in range(16):
                s = t % 3
                S.dma("sp", lambda e, t=t, s=s: e.dma_start(out=xs3[s], in_=xl[128 * t:128 * (t + 1), :]),
                      writes=[b_xs3[s]])
                for hf in range(2):
                    S.op("dve", lambda e, t=t, s=s, hf=hf: e.tensor_copy(
                        out=x1[:, t, 512 * hf:512 * (hf + 1)], in_=xs3[s][:, 512 * hf:512 * (hf + 1)]),
                        reads=[b_xs3[s]], writes=[b_x1[t][hf]])

        S.barrier()

        def dump_x1():
            for t in range(16):
                S.dma("sp", lambda e, t=t: e.dma_start(out=dbg_o[128 * t:128 * (t + 1), :], in_=x1[:, t, :]),
                      reads=b_x1[t], sem="d_dbg")

        if dbg and stage <= 2:
            dump_x1()

        A.top = r2_off
        hT2 = A.alloc((8, NTOK), BF16)
        NSLOT = 3
        wg_b = [A.alloc((8, 512), BF16) for _ in range(2)]
        wu_b = [A.alloc((8, 512), BF16) for _ in range(2)]
        wd_b = [A.alloc((4, 1024), BF16) for _ in range(2)]
        sg = [A.alloc((512,), F32) for _ in range(2)]
        hid = [A.alloc((4, 512), BF16) for _ in range(2)]
        m1_mark = A.top
        xnf = [A.alloc((1024,), F32) for _ in range(2)]
        h2f = [A.alloc((8, 128), F32) for _ in range(2)]
        junkm = A.alloc((1024,), BF16)
        rt = A.alloc((16 * 136,), F32)
        b_hT2 = [Buf("hT2_%d" % i) for i in range(4)]
        b_wg = [Buf("wg%d" % i) for i in range(NSLOT)]
        b_wu = [Buf("wu%d" % i) for i in range(NSLOT)]
        b_wd = [Buf("wd%d" % i) for i in range(NSLOT)]
        b_sg = [Buf("sg0"), Buf("sg1")]
        b_hid = [Buf("hid0"), Buf("hid1")]
        b_xnf = [Buf("xnf0"), Buf("xnf1")]
        b_h2f = [Buf("h2f0"), Buf("h2f1")]
        b_junkm = Buf("junkm")
        b_rt = Buf("rt")

        def load_expert(ei):
            sl = ei % NSLOT
            al = m1_alias if sl == 2 else []
            S.dma("pool", lambda e: e.dma_start(out=wg_b[sl], in_=w_eg[ei].rearrange("(k p) f -> p k f", p=128)),
                  writes=[b_wg[sl]] + al)
            S.dma("pool", lambda e: e.dma_start(out=wu_b[sl], in_=w_eu[ei].rearrange("(k p) f -> p k f", p=128)),
                  writes=[b_wu[sl]] + al)
            S.dma("pool", lambda e: e.dma_start(out=wd_b[sl], in_=w_ed[ei].rearrange("(k p) f -> p k f", p=128)),
                  writes=[b_wd[sl]] + al)

        NEXP = 32 if stage >= 3 else 0
        m1_alias = [b_xnf[0], b_xnf[1], b_h2f[0], b_h2f[1], b_junkm, b_rt]
        for ei in range(min(2, NEXP)):
            load_expert(ei)

        if stage >= 3:
            rstd_ops(stm[:, 0, :], stm[:, 1, :], stm[:, 2, :], 1024, [b_stm])

            def m1_s0(t):
                s = t % 2
                S.op("dve", lambda e: e.tensor_scalar(out=xnf[s], in0=x1[:, t, :], scalar1=stm[:, 2, t:t + 1],
                                                      scalar2=None, op0=ALU.mult),
                     reads=b_x1[t] + [b_stm], writes=[b_xnf[s]])
                pb0 = 2 * (t % 2)
                for k in range(8):
                    bk = pb0 + k // 4
                    S.op("pe", lambda e, k=k, bk=bk: e.transpose(
                        out=psb(bk)[:, 128 * (k % 4):128 * (k % 4 + 1)], in_=xnf[s][:, 128 * k:128 * (k + 1)],
                        identity=ident_f), reads=[b_xnf[s], b_ident], writes=[b_ps[bk]])

            def m1_s1(t):
                s = t % 2
                pb0 = 2 * (t % 2)
                for hh in range(2):
                    bk = pb0 + hh
                    S.op("dve", lambda e, bk=bk, hh=hh: e.tensor_tensor(
                        out=h2f[s][:, 4 * hh:4 * hh + 4, :], in0=psb(bk).rearrange("p (k t) -> p k t", k=4),
                        in1=gT[:, 8 + 4 * hh:12 + 4 * hh].unsqueeze(2).to_broadcast([128, 4, 128]), op=ALU.mult),
                        reads=[b_ps[bk], b_gT], writes=[b_h2f[s]])
                S.op("pool", lambda e: e.tensor_copy(out=hT2[:, :, 128 * t:128 * (t + 1)], in_=h2f[s]),
                     reads=[b_h2f[s]], writes=[b_hT2[t // 4]])
                lb = 4 + t // 8
                c0 = 36 * (t % 8)
                for k in range(8):
                    S.op("pe", lambda e, k=k: e.matmul(
                        psb(lb)[:, c0:c0 + 36], lhsT=h2f[s][:, k, :], rhs=wr[:, k, :], start=(k == 0), stop=(k == 7)),
                        reads=[b_h2f[s], b_wr], writes=[b_ps[lb]])

            pipeline(16, [m1_s0, m1_s1])

            lg = rt[:, 0:576].rearrange("p (t c) -> p t c", t=16)
            gl = lg[:, :, 0:4]
            el = lg[:, :, 4:36].rearrange("p t (g e) -> p t g e", g=4)
            o_ = [576]

            def carve(n):
                v = rt[:, o_[0]:o_[0] + n]
                o_[0] += n
                return v

            gmax, gsum, gw, m1v, m2v, dm, ex, den_, w1, w2 = [carve(16) for _ in range(10)]
            goh = carve(64).rearrange("p (t g) -> p t g", t=16)
            gsh = carve(64).rearrange("p (t g) -> p t g", t=16)
            tmp = carve(512).rearrange("p (t g e) -> p t g e", t=16, g=4)
            esel = carve(128).rearrange("p (t e) -> p t e", t=16)
            oh1 = carve(128).rearrange("p (t e) -> p t e", t=16)
            e2 = carve(128).rearrange("p (t e) -> p t e", t=16)
            oh2 = carve(128).rearrange("p (t e) -> p t e", t=16)
            we = carve(128).rearrange("p (t e) -> p t e", t=16)
            we2 = carve(128).rearrange("p (t e) -> p t e", t=16)
            assert o_[0] <= 16 * 136
            rb = [b_rt]

            def D(fn, extra_r=(), extra_w=()):
                S.op("dve", fn, reads=rb + list(extra_r), writes=rb + list(extra_w))

            def bc3(v, n):
                return v.unsqueeze(2).to_broadcast([128, 16, n])

            for hb in range(2):
                D(lambda e, hb=hb: e.tensor_tensor(
                    out=lg[:, 8 * hb:8 * hb + 8, :], in0=psb(4 + hb)[:, 0:288].rearrange("p (t c) -> p t c", t=8),
                    in1=rbias.unsqueeze(1).to_broadcast([128, 8, 36]), op=ALU.add), [b_ps[4 + hb], b_rbias])
            D(lambda e: e.reduce_max(out=gmax, in_=gl, axis=AX.X))
            D(lambda e: e.tensor_tensor(out=goh, in0=gl, in1=bc3(gmax, 4), op=ALU.is_equal))
            D(lambda e: e.tensor_tensor(out=gsh, in0=gl, in1=bc3(gmax, 4), op=ALU.subtract))
            S.op("act", lambda e: e.activation(out=gsh, in_=gsh, func=AF.Exp), reads=rb, writes=rb)
            D(lambda e: e.reduce_sum(out=gsum, in_=gsh, axis=AX.X))
            D(lambda e: e.reciprocal(out=gw, in_=gsum))
            D(lambda e: e.tensor_tensor(out=tmp, in0=el, in1=goh.unsqueeze(3).to_broadcast([128, 16, 4, 8]),
                                        op=ALU.mult))
            D(lambda e: e.tensor_reduce(out=esel, in_=tmp.rearrange("p t g e -> p t e g"), axis=AX.X, op=ALU.add))
            D(lambda e: e.reduce_max(out=m1v, in_=esel, axis=AX.X))
            D(lambda e: e.tensor_tensor(out=oh1, in0=esel, in1=bc3(m1v, 8), op=ALU.is_equal))
            D(lambda e: e.scalar_tensor_tensor(out=e2, in0=oh1, scalar=-1e30, in1=esel, op0=ALU.mult, op1=ALU.add))
            D(lambda e: e.reduce_max(out=m2v, in_=e2, axis=AX.X))
            D(lambda e: e.tensor_tensor(out=oh2, in0=e2, in1=bc3(m2v, 8), op=ALU.is_equal))
            D(lambda e: e.tensor_tensor(out=dm, in0=m2v, in1=m1v, op=ALU.subtract))
            S.op("act", lambda e: e.activation(out=ex, in_=dm, func=AF.Exp), reads=rb, writes=rb)
            D(lambda e: e.tensor_scalar(out=den_, in0=ex, scalar1=1.0, scalar2=None, op0=ALU.add))
            D(lambda e: e.reciprocal(out=den_, in_=den_))
            D(lambda e: e.tensor_tensor(out=w1, in0=den_, in1=gw, op=ALU.mult))
            D(lambda e: e.tensor_tensor(out=w2, in0=w1, in1=ex, op=ALU.mult))
            D(lambda e: e.tensor_tensor(out=we, in0=oh1, in1=bc3(w1, 8), op=ALU.mult))
            D(lambda e: e.tensor_tensor(out=we2, in0=oh2, in1=bc3(w2, 8), op=ALU.mult))
            D(lambda e: e.tensor_tensor(out=we, in0=we, in1=we2, op=ALU.add))
            D(lambda e: e.tensor_tensor(
                out=dw.rearrange("p t (g e) -> p t g e", g=4),
                in0=goh.unsqueeze(3).to_broadcast([128, 16, 4, 8]),
                in1=we.unsqueeze(2).to_broadcast([128, 16, 4, 8]), op=ALU.mult), (), b_dw)

        A.top = m1_mark
        wg_b.append(A.alloc((8, 512), BF16))
        wu_b.append(A.alloc((8, 512), BF16))
        wd_b.append(A.alloc((4, 1024), BF16))
        ucount = [0]
        wpg_b = A.alloc((8, 1024), BF16)
        b_wpg = Buf("wpg")

        def p_stats(tb):
            if stage >= 4:
                for t in range(4 * tb, 4 * tb + 4):
                    S.op("act", lambda e, t=t: e.activation(out=junk4, in_=x1[:, t, :], func=AF.Square,
                                                            accum_out=st4[:, 0, t:t + 1]),
                         reads=b_x1[t], writes=[b_junk4, b_st4])
        GB = (0, 1)
        UB = (2, 3)
        DB = (4, 5, 6, 7)
        grot = [0]
        drot = [0]
        mpend = [None]

        def expert_unit(ei, tb):
            sl = ei % NSLOT
            hi = ucount[0] % 2
            ucount[0] += 1

            def front():
                for f in range(4):
                    gi = grot[0] % 2
                    grot[0] += 1
                    gb, ub = GB[gi], UB[gi]
                    for k in range(8):
                        S.op("pe", lambda e, k=k, f=f, gb=gb: e.matmul(
                            psb(gb), lhsT=wg_b[sl][:, k, 128 * f:128 * (f + 1)],
                            rhs=hT2[:, k, 512 * tb:512 * (tb + 1)], start=(k == 0), stop=(k == 7)),
                            reads=[b_wg[sl], b_hT2[tb]], writes=[b_ps[gb]])
                    for k in range(8):
                        S.op("pe", lambda e, k=k, f=f, ub=ub: e.matmul(
                            psb(ub), lhsT=wu_b[sl][:, k, 128 * f:128 * (f + 1)],
                            rhs=hT2[:, k, 512 * tb:512 * (tb + 1)], start=(k == 0), stop=(k == 7)),
                            reads=[b_wu[sl], b_hT2[tb]], writes=[b_ps[ub]])
                    S.op("act", lambda e, gb=gb, gi=gi: e.activation(out=sg[gi], in_=psb(gb), func=AF.Silu),
                         reads=[b_ps[gb]], writes=[b_sg[gi]])
                    S.op("dve", lambda e, ub=ub, gi=gi, f=f: e.tensor_tensor(
                        out=hid[hi][:, f, :], in0=sg[gi], in1=psb(ub), op=ALU.mult),
                        reads=[b_sg[gi], b_ps[ub]], writes=[b_hid[hi]])

            def back():
                for tt in range(4):
                    t = 4 * tb + tt
                    for hf in range(2):
                        db = DB[drot[0] % 4]
                        drot[0] += 1
                        for fk in range(4):
                            S.op("pe", lambda e, fk=fk, tt=tt, hf=hf, db=db: e.matmul(
                                psb(db), lhsT=hid[hi][:, fk, 128 * tt:128 * (tt + 1)],
                                rhs=wd_b[sl][:, fk, 512 * hf:512 * (hf + 1)], start=(fk == 0), stop=(fk == 3)),
                                reads=[b_hid[hi], b_wd[sl]], writes=[b_ps[db]])
                        S.op("dve", lambda e, t=t, hf=hf, db=db: e.scalar_tensor_tensor(
                            out=x1[:, t, 512 * hf:512 * (hf + 1)], in0=psb(db), scalar=dw[:, t, ei:ei + 1],
                            in1=x1[:, t, 512 * hf:512 * (hf + 1)], op0=ALU.mult, op1=ALU.add),
                            reads=[b_ps[db], b_dw[t], b_x1[t][hf]], writes=[b_x1[t][hf]])

            front()
            if mpend[0] is not None:
                mpend[0]()
            mpend[0] = back

        for ei in range(NEXP):
            for tb in range(4):
                expert_unit(ei, tb)
                if tb == 0 and ei + NSLOT - 1 < NEXP:
                    load_expert(ei + NSLOT - 1)
                if tb == 0 and ei == NEXP - 2:
                    S.dma("pool", lambda e: e.dma_start(out=wpg_b, in_=w_pg.rearrange("(k p) c -> p k c", p=128)),
                          writes=[b_wpg] + m1_alias)
                if ei == NEXP - 1 and tb >= 1:
                    p_stats(tb - 1)
        if mpend[0] is not None:
            mpend[0]()
            mpend[0] = None
        if NEXP > 0:
            p_stats(3)

        S.barrier()
        if dbg and stage == 3:
            dump_x1()

        A.top = r2_off
        gfin = A.alloc((1024,), F32)
        S.dma("sp", lambda e: e.dma_start(out=gfin, in_=g_final.partition_broadcast(128)), writes=[b_gfin])
        wpp_b = A.alloc((2, 1024), BF16)
        pbf = [A.alloc((256,), BF16) for _ in range(2)]
        pT = [A.alloc((2, 128), BF16) for _ in range(2)]
        xn4 = [A.alloc((1024,), BF16) for _ in range(2)]
        hT3 = [A.alloc((8, 128), BF16) for _ in range(2)]
        sgm = [A.alloc((512,), F32) for _ in range(2)]
        yt = [A.alloc((1024,), F32) for _ in range(3)]
        b_wpp = Buf("wpp")
        b_pbf = [Buf("pbf0"), Buf("pbf1")]
        b_pT = [Buf("pT0"), Buf("pT1")]
        b_xn4 = [Buf("xn4_0"), Buf("xn4_1")]
        b_hT3 = [Buf("hT3_0"), Buf("hT3_1")]
        b_sgm = [Buf("sgm0"), Buf("sgm1")]
        b_yt = [Buf("yt%d" % i) for i in range(3)]
        b_st4f = [Buf("st4f%d" % g) for g in range(4)]

        if NEXP < 2:
            S.dma("pool", lambda e: e.dma_start(out=wpg_b, in_=w_pg.rearrange("(k p) c -> p k c", p=128)),
                  writes=[b_wpg])
        S.dma("pool", lambda e: e.dma_start(out=wpp_b, in_=w_pp.rearrange("(k p) c -> p k c", p=128)),
              writes=[b_wpp])
        gpb = gT[:, 16:24].unsqueeze(2).to_broadcast([128, 8, 128])
        ycount = [0]

        def final_group(g):
            ts_ = list(range(4 * g, 4 * g + 4))
            for t in ts_:
                S.op("act", lambda e, t=t: e.activation(out=junk4, in_=x1[:, t, :], func=AF.Square,
                                                        accum_out=st4[:, 3, t:t + 1]),
                     reads=b_x1[t], writes=[b_junk4, b_st4f[g]])
            sl_ = slice(4 * g, 4 * g + 4)
            rstd_ops(st4[:, 3, sl_], st4[:, 4, sl_], st4[:, 5, sl_], 1024, [b_st4f[g]])
            for t in ts_:
                s = ycount[0] % 3
                ycount[0] += 1
                S.op("dve", lambda e, t=t, s=s: e.scalar_tensor_tensor(
                    out=yt[s], in0=x1[:, t, :], scalar=st4[:, 5, t:t + 1], in1=gfin, op0=ALU.mult, op1=ALU.mult),
                    reads=b_x1[t] + [b_st4f[g], b_gfin], writes=[b_yt[s]])
                S.dma("sp", lambda e, t=t, s=s: e.dma_start(out=y[128 * t:128 * (t + 1), :], in_=yt[s]),
                      reads=[b_yt[s]], sem="d_y%d" % s)

        if stage >= 4:
            if NEXP == 0:
                for t in range(16):
                    S.op("act", lambda e, t=t: e.activation(out=junk4, in_=x1[:, t, :], func=AF.Square,
                                                            accum_out=st4[:, 0, t:t + 1]),
                         reads=b_x1[t], writes=[b_junk4, b_st4])
            rstd_ops(st4[:, 0, :], st4[:, 1, :], st4[:, 2, :], 1024, [b_st4])
            ppend = [None]

            def p_s0(t):
                s = t % 2
                S.dma("pool", lambda e: e.dma_start(out=pbf[s], in_=pl[128 * t:128 * (t + 1), :]), writes=[b_pbf[s]])
                S.op("dve", lambda e: e.tensor_scalar(out=xn4[s], in0=x1[:, t, :], scalar1=st4[:, 2, t:t + 1],
                                                      scalar2=None, op0=ALU.mult),
                     reads=b_x1[t] + [b_st4], writes=[b_xn4[s]])
                pb = t % 2
                for k in range(8):
                    S.op("pe", lambda e, k=k: e.transpose(
                        out=psb16(pb)[:, 128 * k:128 * (k + 1)], in_=xn4[s][:, 128 * k:128 * (k + 1)],
                        identity=ident_b), reads=[b_xn4[s], b_ident], writes=[b_ps[pb]])
                tp = 2 + t % 2
                for c in range(2):
                    S.op("pe", lambda e, c=c: e.transpose(
                        out=psb16(tp)[:, 128 * c:128 * (c + 1)], in_=pbf[s][:, 128 * c:128 * (c + 1)],
                        identity=ident_b), reads=[b_pbf[s], b_ident], writes=[b_ps[tp]])

            def p_s1(t):
                s = t % 2
                pb = t % 2
                tp = 2 + t % 2
                S.op("dve", lambda e: e.tensor_tensor(
                    out=hT3[s], in0=psb16(pb).rearrange("p (k t) -> p k t", k=8), in1=gpb, op=ALU.mult),
                    reads=[b_ps[pb], b_gT], writes=[b_hT3[s]])
                S.op("act", lambda e: e.copy(
                    out=pT[s], in_=psb16(tp)[:, 0:256].rearrange("p (c t) -> p c t", c=2)),
                    reads=[b_ps[tp]], writes=[b_pT[s]])
                for hf in range(2):
                    u = 2 * t + hf
                    gbk = 4 + 2 * (u % 2)
                    pbk = 5 + 2 * (u % 2)
                    for k in range(8):
                        S.op("pe", lambda e, k=k, hf=hf, gbk=gbk: e.matmul(
                            psb(gbk), lhsT=hT3[s][:, k, :], rhs=wpg_b[:, k, 512 * hf:512 * (hf + 1)],
                            start=(k == 0), stop=(k == 7)),
                            reads=[b_hT3[s], b_wpg], writes=[b_ps[gbk]])
                    for c in range(2):
                        S.op("pe", lambda e, c=c, hf=hf, pbk=pbk: e.matmul(
                            psb(pbk), lhsT=pT[s][:, c, :], rhs=wpp_b[:, c, 512 * hf:512 * (hf + 1)],
                            start=(c == 0), stop=(c == 1)),
                            reads=[b_pT[s], b_wpp], writes=[b_ps[pbk]])

                    def tail(t=t, hf=hf, u=u, gbk=gbk, pbk=pbk):
                        sm = u % 2
                        S.op("act", lambda e: e.activation(out=sgm[sm], in_=psb(gbk), func=AF.Sigmoid),
                             reads=[b_ps[gbk]], writes=[b_sgm[sm]])
                        S.op("dve", lambda e: e.tensor_tensor(out=sgm[sm], in0=sgm[sm], in1=psb(pbk), op=ALU.mult),
                             reads=[b_sgm[sm], b_ps[pbk]], writes=[b_sgm[sm]])
                        S.op("dve", lambda e: e.tensor_tensor(
                            out=x1[:, t, 512 * hf:512 * (hf + 1)], in0=x1[:, t, 512 * hf:512 * (hf + 1)],
                            in1=sgm[sm], op=ALU.add), reads=[b_sgm[sm], b_x1[t][hf]], writes=[b_x1[t][hf]])
                        if hf == 1 and t % 4 == 3:
                            final_group(t // 4)

                    if ppend[0] is not None:
                        ppend[0]()
                    ppend[0] = tail

            pipeline(16, [p_s0, p_s1])
            if ppend[0] is not None:
                ppend[0]()
                ppend[0] = None
        else:
            for g in range(4):
                final_group(g)

        S.barrier()
        S.emit()
    return nc


_NC_CACHE = {}


def _get_nc(stage, dbg):
    key = (stage, dbg)
    if key not in _NC_CACHE:
        _NC_CACHE[key] = build_nc(stage, dbg)
    return _NC_CACHE[key]


def kernel(x, p, g_mix, w_in, sink, g_grp_a, g_grp_b, w_out, g_ffn,
           w_router_group, b_router_group, w_router_expert, b_router_expert,
           w_expert_gate, w_expert_up, w_expert_down, g_ple, w_ple_gate, w_ple_proj,
           g_final, _stage=99, _dbg=False):
    f = lambda a: np.ascontiguousarray(np.asarray(a, dtype=np.float32))
    x = f(x)
    p = f(p)
    shared = {
        "g_mix": f(g_mix[0]), "w_in": f(w_in[0]), "sink": f(sink[0]),
        "g_grp_a": f(g_grp_a[0]), "g_grp_b": f(g_grp_b[0]), "w_out": f(w_out[0]),
        "g_ffn": f(g_ffn[0]), "w_rg": f(w_router_group[0]), "b_rg": f(b_router_group[0]),
        "w_re": f(w_router_expert[0]), "b_re": f(b_router_expert[0]),
        "w_eg": f(w_expert_gate[0]), "w_eu": f(w_expert_up[0]), "w_ed": f(w_expert_down[0]),
        "g_ple": f(g_ple[0]), "w_pg": f(w_ple_gate[0]), "w_pp": f(w_ple_proj[0]),
        "g_final": f(g_final),
    }
    if _stage < 3:
        for k_ in ("w_eg", "w_eu", "w_ed"):
            shared.pop(k_)
    in_maps = []
    for c in range(8):
        b, hf = c // 2, c % 2
        if hf == 0:
            xl = x[b, 0:NLOC]
            plc = p[0, b, 0:NTOK]
        else:
            xl = x[b, 4096 - NLOC:4096][::-1]
            plc = p[0, b, NTOK:4096][::-1]
        m = dict(shared)
        m["xl"] = np.ascontiguousarray(xl)
        m["pl"] = np.ascontiguousarray(plc)
        in_maps.append(m)
    nc = _get_nc(_stage, _dbg)
    res = run_bass_kernel_spmd(nc, in_maps, core_ids=list(range(8)))
    out = np.empty((4, 4096, 1024), np.float32)
    dbg = np.empty((4, 4096, 1024), np.float32) if _dbg else None
    for c in range(8):
        b, hf = c // 2, c % 2
        yc = res.results[c]["y"]
        if hf == 0:
            out[b, 0:NTOK] = yc
        else:
            out[b, NTOK:4096] = yc[::-1]
        if _dbg:
            dc = res.results[c]["dbg"]
            if hf == 0:
                dbg[b, 0:NTOK] = dc
            else:
                dbg[b, NTOK:4096] = dc[::-1]
    if _dbg:
        return out, dbg
    return out
```
